# Optimizing a Trainium2 kernel written in Bass

```python
import math
import jax, jax.numpy as jnp
from jax import lax
import numpy as np

D_MODEL = 2048
BATCH = 16
SEQ = 256
DEPTH = 2
DEC_BATCH = 2
DEC_SEQ = 4096
PAST_LEN = 256

GRID_W = 64
N_MIXERS = 2
EPS = 1e-6
DN_HK = 16
DN_HV = 32
DN_DK = 128
DN_DV = 128
DN_CONV = 5
DN_CHUNK = 64
DN_QK_W = DN_HK * DN_DK
DN_V_W = DN_HV * DN_DV
DN_QKV = 2 * DN_QK_W + DN_V_W
DN_IN = DN_QKV + DN_V_W + 4 * DN_HV
NA_HEADS = 16
NA_HD = D_MODEL // NA_HEADS
WIN_R = 8
WIN_C = 16
CTX_QBLOCK = 128
PEER_HEADS = 8
PEER_NKEYS = 128
PEER_N = PEER_NKEYS * PEER_NKEYS
PEER_DQ = 256
PEER_TOPK = 16
PEER_TBLOCK = 128

kernel_name = 'hybrid_deltanet_natten_peer_diffusion_step'


def _rmsnorm(x, g):
    x32 = x.astype(jnp.float32)
    y = x32 * lax.rsqrt(jnp.mean(x32 * x32, axis=-1, keepdims=True) + EPS)
    return (y * g.astype(jnp.float32)).astype(x.dtype)


def _l2norm(x):
    x32 = x.astype(jnp.float32)
    return (x32 * lax.rsqrt(jnp.sum(x32 * x32, axis=-1, keepdims=True) + EPS)).astype(x.dtype)


def _modulate(x, shift, scale):
    return x * (1 + scale) + shift


def _adaln(cvec, w, b):
    m = jax.nn.silu(cvec) @ w + b
    if m.ndim == 2:
        m = m[:, None, :]
    return jnp.split(m, 6, axis=-1)


def _short_conv(x, w):
    ch = x.shape[-1]
    return lax.conv_general_dilated(
        x, w[:, None, :].astype(x.dtype), window_strides=(1,),
        padding=[(DN_CONV // 2, DN_CONV // 2)],
        dimension_numbers=('NWC', 'WIO', 'NWC'), feature_group_count=ch)


def _gated_delta_chunked(q, k, v, beta, g, s0):
    f32 = jnp.float32
    bsz, seq, nh, dk = q.shape
    dv = v.shape[-1]
    n = seq // DN_CHUNK

    def blk(t):
        return t.astype(f32).reshape(bsz, n, DN_CHUNK, nh, -1).transpose(1, 0, 3, 2, 4)

    def blk_s(t):
        return t.astype(f32).reshape(bsz, n, DN_CHUNK, nh).transpose(1, 0, 3, 2)

    q, k, v = blk(q), blk(k), blk(v)
    beta, g = blk_s(beta), blk_s(g)
    G = jnp.cumsum(g, axis=-1)
    idx = jnp.arange(DN_CHUNK)
    incl = idx[:, None] >= idx[None, :]
    strict = idx[:, None] > idx[None, :]
    decay = jnp.exp(jnp.where(incl, G[..., :, None] - G[..., None, :], -jnp.inf))
    kk = jnp.einsum('nbhid,nbhjd->nbhij', k, k)
    A = jnp.where(strict, beta[..., None] * decay * kk, 0.0)
    M = jnp.eye(DN_CHUNK, dtype=f32) + A
    rhs = jnp.concatenate([beta[..., None] * v, (beta * jnp.exp(G))[..., None] * k], axis=-1)
    sol = lax.linalg.triangular_solve(M, rhs, left_side=True, lower=True, unit_diagonal=True)
    u, wk = sol[..., :dv], sol[..., dv:]
    qk = decay * jnp.einsum('nbhid,nbhjd->nbhij', q, k)
    qg = q * jnp.exp(G)[..., None]
    kdec = k * jnp.exp(G[..., -1:] - G)[..., None]
    glast = jnp.exp(G[..., -1])

    def step(S, xs):
        u_c, wk_c, qk_c, qg_c, kd_c, gl_c = xs
        w = u_c - jnp.einsum('bhik,bhkv->bhiv', wk_c, S)
        o = jnp.einsum('bhik,bhkv->bhiv', qg_c, S) + jnp.einsum('bhij,bhjv->bhiv', qk_c, w)
        S = gl_c[..., None, None] * S + jnp.einsum('bhik,bhiv->bhkv', kd_c, w)
        return S, o

    S, o = lax.scan(step, s0.astype(f32), (u, wk, qk, qg, kdec, glast))
    o = o.transpose(1, 0, 3, 2, 4).reshape(bsz, seq, nh, dv)
    return o, S


def _deltanet(h, w_in, conv_w, a_log, dt_bias, norm_g, w_o, s0):
    f32 = jnp.float32
    bsz, seq, _ = h.shape
    proj = h @ w_in
    qkv = jax.nn.silu(_short_conv(proj[..., :DN_QKV], conv_w))
    z = proj[..., DN_QKV:DN_QKV + DN_V_W].reshape(bsz, seq, DN_HV, DN_DV).astype(f32)
    ba = proj[..., DN_QKV + DN_V_W:].astype(f32).reshape(bsz, seq, 2, 2, DN_HV)
    rep = DN_HV // DN_HK
    q = jnp.repeat(_l2norm(qkv[..., :DN_QK_W].reshape(bsz, seq, DN_HK, DN_DK)), rep, axis=2) * (DN_DK ** -0.5)
    k = jnp.repeat(_l2norm(qkv[..., DN_QK_W:2 * DN_QK_W].reshape(bsz, seq, DN_HK, DN_DK)), rep, axis=2)
    v = qkv[..., 2 * DN_QK_W:].reshape(bsz, seq, DN_HV, DN_DV)
    beta = jax.nn.sigmoid(ba[:, :, 0])
    g = -jnp.exp(a_log.astype(f32)) * jax.nn.softplus(ba[:, :, 1] + dt_bias.astype(f32))
    o_f, s_f = _gated_delta_chunked(q, k, v, beta[:, :, 0], g[:, :, 0], s0[:, 0])
    flip = lambda t: jnp.flip(t, axis=1)
    o_b, s_b = _gated_delta_chunked(flip(q), flip(k), flip(v), flip(beta[:, :, 1]), flip(g[:, :, 1]), s0[:, 1])
    o = o_f + flip(o_b)
    o = o * lax.rsqrt(jnp.mean(o * o, axis=-1, keepdims=True) + EPS) * norm_g.astype(f32) * jax.nn.silu(z)
    out = o.astype(h.dtype).reshape(bsz, seq, DN_V_W) @ w_o
    return out, jnp.stack([s_f, s_b], axis=1)


def _na_qkv(h, w_qkv):
    bsz, seq, _ = h.shape
    qkv = (h @ w_qkv).reshape(bsz, seq, 3, NA_HEADS, NA_HD)
    return qkv[:, :, 0] * (NA_HD ** -0.5), qkv[:, :, 1], qkv[:, :, 2]


def _na_context(h, w_qkv, w_o):
    bsz, seq, d = h.shape
    q, k, v = _na_qkv(h, w_qkv)
    nb = seq // CTX_QBLOCK
    qb = q.reshape(bsz, nb, CTX_QBLOCK, NA_HEADS, NA_HD).transpose(1, 0, 2, 3, 4)

    def blk(qq):
        s = jnp.einsum('bqhd,bchd->bhqc', qq, k).astype(jnp.float32)
        p = jax.nn.softmax(s, axis=-1).astype(v.dtype)
        return jnp.einsum('bhqc,bchd->bqhd', p, v)

    o = lax.map(blk, qb).transpose(1, 0, 2, 3, 4).reshape(bsz, seq, d)
    return o @ w_o, k, v


def _na_latent(h, w_qkv, rpb, w_o, k_ctx, v_ctx):
    f32 = jnp.float32
    bsz, seq, d = h.shape
    rows = seq // GRID_W
    wr = min(WIN_R, rows)
    n_lat = wr * GRID_W
    q, k, v = _na_qkv(h, w_qkv)
    qg = q.reshape(bsz, rows, GRID_W, NA_HEADS, NA_HD)
    kg = k.reshape(bsz, rows, GRID_W, NA_HEADS, NA_HD)
    vg = v.reshape(bsz, rows, GRID_W, NA_HEADS, NA_HD)
    col = jnp.arange(GRID_W)
    c0 = jnp.clip(col - WIN_C // 2, 0, GRID_W - WIN_C)
    col_ok = (col[None, :] >= c0[:, None]) & (col[None, :] < c0[:, None] + WIN_C)
    dc_idx = jnp.clip(col[None, :] - col[:, None], 1 - WIN_C, WIN_C - 1) + WIN_C - 1

    def row_step(r):
        r0 = jnp.clip(r - wr // 2, 0, rows - wr)
        q_r = lax.dynamic_index_in_dim(qg, r, axis=1, keepdims=False)
        k_w = lax.dynamic_slice_in_dim(kg, r0, wr, axis=1)
        v_w = lax.dynamic_slice_in_dim(vg, r0, wr, axis=1).reshape(bsz, n_lat, NA_HEADS, NA_HD)
        s_lat = jnp.einsum('bqhd,bwkhd->bhqwk', q_r, k_w).astype(f32)
        dr_idx = r0 + jnp.arange(wr) - r + WIN_R - 1
        bias = rpb[:, dr_idx[:, None, None], dc_idx[None, :, :]].transpose(0, 2, 1, 3).astype(f32)
        s_lat = jnp.where(col_ok[:, None, :], s_lat + bias, -jnp.inf).reshape(bsz, NA_HEADS, GRID_W, n_lat)
        s_ctx = jnp.einsum('bqhd,bchd->bhqc', q_r, k_ctx).astype(f32)
        p = jax.nn.softmax(jnp.concatenate([s_lat, s_ctx], axis=-1), axis=-1).astype(v_w.dtype)
        return (jnp.einsum('bhqn,bnhd->bqhd', p[..., :n_lat], v_w)
                + jnp.einsum('bhqc,bchd->bqhd', p[..., n_lat:], v_ctx))

    o = lax.map(row_step, jnp.arange(rows))
    o = o.transpose(1, 0, 2, 3, 4).reshape(bsz, seq, d)
    return o @ w_o


def _peer(h, w_q, keys, u_tab, v_tab):
    bsz, seq, d = h.shape
    t = bsz * seq
    x = h.reshape(t, d)
    q = (x @ w_q).reshape(t, PEER_HEADS, 2, PEER_DQ // 2)
    s = jnp.einsum('thpd,hpnd->thpn', q, keys).astype(jnp.float32)
    s1, i1 = lax.top_k(s[:, :, 0], PEER_TOPK)
    s2, i2 = lax.top_k(s[:, :, 1], PEER_TOPK)
    cand = (s1[..., :, None] + s2[..., None, :]).reshape(t, PEER_HEADS, PEER_TOPK * PEER_TOPK)
    best, pos = lax.top_k(cand, PEER_TOPK)
    eid = (jnp.take_along_axis(i1, pos // PEER_TOPK, axis=-1) * PEER_NKEYS
           + jnp.take_along_axis(i2, pos % PEER_TOPK, axis=-1))
    gate = jax.nn.softmax(best, axis=-1).astype(h.dtype)
    nsel = PEER_HEADS * PEER_TOPK
    nb = t // PEER_TBLOCK

    def blk(args):
        xb, eb, gb = args
        u = jnp.take(u_tab, eb, axis=0)
        a = jax.nn.gelu(jnp.einsum('tkd,td->tk', u, xb), approximate=False) * gb
        vv = jnp.take(v_tab, eb, axis=0)
        return jnp.einsum('tk,tkd->td', a, vv)

    out = lax.map(blk, (x.reshape(nb, PEER_TBLOCK, d), eid.reshape(nb, PEER_TBLOCK, nsel),
                        gate.reshape(nb, PEER_TBLOCK, nsel)))
    return out.reshape(bsz, seq, d)


def setup_inputs(seed: int = 0) -> dict:
    key = jax.random.key(seed)
    ks = jax.random.split(key, 32)
    f32 = jnp.float32

    def nrm(i, shape, s):
        return jax.random.normal(ks[i], shape, f32) * s

    D = D_MODEL
    n_dn = (DEPTH + 1) // 2
    n_na = DEPTH // 2
    dt = jnp.exp(jax.random.uniform(ks[20], (n_dn, 2, DN_HV), f32, minval=math.log(1e-3), maxval=math.log(1e-1)))
    return {
        'x_prompt': nrm(0, (BATCH, SEQ, D), 1.0),
        'x_sample': nrm(1, (DEC_BATCH, DEC_SEQ, D), 1.0),
        'c': nrm(2, (DEC_BATCH, D), 1.0),
        'state_delta': nrm(3, (DEC_BATCH, n_dn, 2, DN_HV, DN_DK, DN_DV), 0.5),
        'cache_k': nrm(4, (DEC_BATCH, n_na, PAST_LEN, NA_HEADS, NA_HD), 1.0),
        'cache_v': nrm(5, (DEC_BATCH, n_na, PAST_LEN, NA_HEADS, NA_HD), 1.0),
        'c_ctx': nrm(6, (D,), 1.0),
        'ada_w': nrm(7, (DEPTH, D, 6 * D), 0.5 * D ** -0.5),
        'ada_b': nrm(8, (DEPTH, 6 * D), 0.02),
        'norm1_g': 1.0 + nrm(9, (DEPTH, D), 0.02),
        'norm2_g': 1.0 + nrm(10, (DEPTH, D), 0.02),
        'final_g': 1.0 + nrm(11, (D,), 0.02),
        'dn_w_in': nrm(12, (n_dn, D, DN_IN), D ** -0.5),
        'dn_conv_w': nrm(13, (n_dn, DN_CONV, DN_QKV), DN_CONV ** -0.5),
        'dn_a_log': jnp.log(jax.random.uniform(ks[14], (n_dn, 2, DN_HV), f32, minval=1.0, maxval=16.0)),
        'dn_dt_bias': dt + jnp.log(-jnp.expm1(-dt)),
        'dn_norm_g': 1.0 + nrm(15, (n_dn, DN_DV), 0.02),
        'dn_w_o': nrm(16, (n_dn, DN_V_W, D), DN_V_W ** -0.5),
        'na_w_qkv': nrm(17, (n_na, D, 3 * D), D ** -0.5),
        'na_rpb': nrm(18, (n_na, NA_HEADS, 2 * WIN_R - 1, 2 * WIN_C - 1), 0.5),
        'na_w_o': nrm(19, (n_na, D, D), D ** -0.5),
        'peer_w_q': nrm(21, (DEPTH, D, PEER_HEADS * PEER_DQ), D ** -0.5),
        'peer_keys': nrm(22, (DEPTH, PEER_HEADS, 2, PEER_NKEYS, PEER_DQ // 2), (PEER_DQ // 2) ** -0.5),
        'peer_u': nrm(23, (DEPTH, PEER_N, D), D ** -0.5),
        'peer_v': nrm(24, (DEPTH, PEER_N, D), 0.5),
    }


def reference(x_prompt, x_sample, c, state_delta, cache_k, cache_v, c_ctx, ada_w, ada_b, norm1_g, norm2_g,
              final_g, dn_w_in, dn_conv_w, dn_a_log, dn_dt_bias, dn_norm_g, dn_w_o, na_w_qkv, na_rpb, na_w_o,
              peer_w_q, peer_keys, peer_u, peer_v):
    xp = x_prompt
    zero_state = jnp.zeros((x_prompt.shape[0], 2, DN_HV, DN_DK, DN_DV), jnp.float32)
    ctx_states, ctx_k, ctx_v = [], [], []
    for i in range(DEPTH):
        j = i // N_MIXERS
        sh1, sc1, g1, sh2, sc2, g2 = _adaln(c_ctx, ada_w[i], ada_b[i])
        h = _modulate(_rmsnorm(xp, norm1_g[i]), sh1, sc1)
        if i % N_MIXERS == 0:
            out, s_fin = _deltanet(h, dn_w_in[j], dn_conv_w[j], dn_a_log[j], dn_dt_bias[j], dn_norm_g[j],
                                   dn_w_o[j], zero_state)
            ctx_states.append(s_fin)
        else:
            out, k_c, v_c = _na_context(h, na_w_qkv[j], na_w_o[j])
            ctx_k.append(k_c)
            ctx_v.append(v_c)
        xp = xp + g1 * out
        h = _modulate(_rmsnorm(xp, norm2_g[i]), sh2, sc2)
        xp = xp + g2 * _peer(h, peer_w_q[i], peer_keys[i], peer_u[i], peer_v[i])
    y_prompt = _rmsnorm(xp, final_g)

    xs = x_sample
    for i in range(DEPTH):
        j = i // N_MIXERS
        sh1, sc1, g1, sh2, sc2, g2 = _adaln(c, ada_w[i], ada_b[i])
        h = _modulate(_rmsnorm(xs, norm1_g[i]), sh1, sc1)
        if i % N_MIXERS == 0:
            out, _ = _deltanet(h, dn_w_in[j], dn_conv_w[j], dn_a_log[j], dn_dt_bias[j], dn_norm_g[j],
                               dn_w_o[j], state_delta[:, j])
        else:
            out = _na_latent(h, na_w_qkv[j], na_rpb[j], na_w_o[j], cache_k[:, j], cache_v[:, j])
        xs = xs + g1 * out
        h = _modulate(_rmsnorm(xs, norm2_g[i]), sh2, sc2)
        xs = xs + g2 * _peer(h, peer_w_q[i], peer_keys[i], peer_u[i], peer_v[i])
    y_sample = _rmsnorm(xs, final_g)

    new_state_delta = jnp.stack(ctx_states, axis=1)
    new_cache_k = jnp.stack(ctx_k, axis=1)
    new_cache_v = jnp.stack(ctx_v, axis=1)
    return (y_prompt, y_sample, new_state_delta, new_cache_k, new_cache_v)
```

```python
from concourse.bass_utils import run_bass_kernel_spmd
import contextlib
import numpy as np
import concourse.bass as bass
import concourse.mybir as mybir

F32 = mybir.dt.float32
BF16 = mybir.dt.bfloat16
I32 = mybir.dt.int32
AF = mybir.ActivationFunctionType
ALU = mybir.AluOpType
AX = mybir.AxisListType


class Em:
    ENG = ("pe", "dve", "act", "pool", "sp")

    def __init__(self, nc, es):
        self.nc = nc
        self.es = es
        self.eng = {"pe": nc.tensor, "dve": nc.vector, "act": nc.scalar, "pool": nc.gpsimd, "sp": nc.sync}
        self.sem = {e: es.enter_context(nc.semaphore("s_" + e)) for e in self.ENG if e != "sp"}
        self.cnt = {e: 0 for e in self.ENG}
        self.seen = {e: {} for e in self.ENG}
        self.last_w = {}
        self.readers = {}
        self.dsem = {}
        self.dfree = []
        self.dscopes = [set()]
        self.n_inst = 0
        self.psum = []
        self.ps_i = 0
        self.uid = 0

    def sb(self, name, shape, dt=F32, es=None):
        self.uid += 1
        return (es or self.es).enter_context(self.nc.sbuf_tensor(f"{name}_{self.uid}", list(shape), dt))

    def init_psum(self):
        for i in range(8):
            self.psum.append(self.es.enter_context(self.nc.psum_tensor(f"ps{i}", [128, 512], F32)))

    def ps(self):
        t = self.psum[self.ps_i % 8]
        self.ps_i += 1
        return t

    @staticmethod
    def key(ap):
        if isinstance(ap, str):
            return ap
        return ap.tensor.name

    def _deps(self, e, reads, writes):
        need = {}
        def add(tok):
            k, v = tok
            if k == "pe" and e == "pe":
                return
            if need.get(k, 0) < v:
                need[k] = v
        for r in reads:
            t = self.last_w.get(r)
            if t:
                add(t)
            if r.startswith("ps"):
                for t in self.readers.get(r, ()):
                    if t[0] != e:
                        add(t)
        for w in writes:
            t = self.last_w.get(w)
            if t:
                add(t)
            for t in self.readers.get(w, ()):
                add(t)
        for k, v in need.items():
            if k.startswith("dma:"):
                v = self.dsem[k[4:]][1]
            if self.seen[e].get(k, 0) < v:
                sem = self.dsem[k[4:]][0] if k.startswith("dma:") else self.sem[k]
                self.eng[e].wait_ge(sem, v)
                self.seen[e][k] = v
                self.n_inst += 1

    def _commit(self, tok, reads, writes):
        for w in writes:
            self.last_w[w] = tok
            self.readers[w] = set()
        for r in reads:
            if r not in writes:
                self.readers.setdefault(r, set()).add(tok)

    def _rw(self, kw, extra_r=(), extra_w=()):
        reads, writes = [], []
        for k, v in kw.items():
            if isinstance(v, bass.AP):
                if k in ("out", "accum_out"):
                    writes.append(self.key(v))
                else:
                    reads.append(self.key(v))
        reads += [self.key(x) for x in extra_r]
        writes += [self.key(x) for x in extra_w]
        return reads, writes

    def op(self, e, name, extra_r=(), extra_w=(), **kw):
        reads, writes = self._rw(kw, extra_r, extra_w)
        self._deps(e, reads, writes)
        inst = getattr(self.eng[e], name)(**kw)
        self.cnt[e] += 1
        inst.then_inc(self.sem[e], 1)
        self.n_inst += 1
        self._commit((e, self.cnt[e]), reads, writes)
        return inst

    def mm(self, out, lhsT, rhs, start=True, stop=True, transpose=False, **kw):
        reads = [self.key(lhsT), self.key(rhs)]
        writes = [self.key(out)]
        self._deps("pe", reads, writes)
        if transpose:
            inst = self.nc.tensor.transpose(out, lhsT, rhs, **kw)
        else:
            inst = self.nc.tensor.matmul(out, lhsT=lhsT, rhs=rhs, start=start, stop=stop, **kw)
        self.n_inst += 1
        if stop:
            self.cnt["pe"] += 1
            inst.then_inc(self.sem["pe"], 1)
            tok = ("pe", self.cnt["pe"])
        else:
            tok = ("pe", self.cnt["pe"] + 1)
        self._commit(tok, reads, writes)
        return inst

    def dma(self, out, in_, stream=None, q="sp", **kw):
        reads = [self.key(in_)]
        writes = [self.key(out)]
        if stream is None:
            stream = self.key(out) if "DRam" not in type(out.tensor).__name__ else self.key(in_)
        if stream not in self.dsem:
            self._new_stream(stream)
        self._deps(q, reads, writes)
        inst = self.eng[q].dma_start(out=out, in_=in_, **kw)
        self.dsem[stream][1] += 16
        inst.then_inc(self.dsem[stream][0], 16)
        self.n_inst += 1
        self._commit(("dma:" + stream, self.dsem[stream][1]), reads, writes)
        return inst

    def _new_stream(self, stream):
        if self.dfree:
            self.dsem[stream] = self.dfree.pop()
        else:
            self.dsem[stream] = [self.es.enter_context(self.nc.semaphore("d%d" % len(self.dsem))), 0]
        self.dscopes[-1].add(stream)

    def barrier(self):
        for e in self.ENG:
            for p in self.sem:
                v = self.cnt[p]
                if v and self.seen[e].get(p, 0) < v:
                    self.eng[e].wait_ge(self.sem[p], v)
                    self.seen[e][p] = v
            for s, (sem, v) in self.dsem.items():
                k = "dma:" + s
                if v and self.seen[e].get(k, 0) < v:
                    self.eng[e].wait_ge(sem, v)
                    self.seen[e][k] = v
        self.last_w.clear()
        self.readers.clear()

    def act(self, out, in_, func, e="act", **kw):
        return self.op(e, "activation", out=out, in_=in_, func=func, **kw)

    def tt(self, out, in0, in1, op, e="dve"):
        return self.op(e, "tensor_tensor", out=out, in0=in0, in1=in1, op=op)

    def ts(self, out, in0, s1, op0, s2=None, op1=None, e="dve", **kw):
        if op1 is None:
            return self.op(e, "tensor_scalar", out=out, in0=in0, scalar1=s1, scalar2=None, op0=op0, **kw)
        return self.op(e, "tensor_scalar", out=out, in0=in0, scalar1=s1, scalar2=s2, op0=op0, op1=op1, **kw)

    def stt(self, out, in0, scalar, in1, op0, op1, e="dve"):
        return self.op(e, "scalar_tensor_tensor", out=out, in0=in0, scalar=scalar, in1=in1, op0=op0, op1=op1)

    def copy(self, out, in_, e="dve"):
        if e == "act":
            return self.op("act", "activation", out=out, in_=in_, func=AF.Copy)
        return self.op(e, "tensor_copy", out=out, in_=in_)

    def memset(self, ap, val, e="dve"):
        reads, writes = [], [self.key(ap)]
        self._deps(e, reads, writes)
        inst = self.eng[e].memset(ap, val)
        self.cnt[e] += 1
        inst.then_inc(self.sem[e], 1)
        self.n_inst += 1
        self._commit((e, self.cnt[e]), reads, writes)
        return inst


def _dbg(self, name, ap, dt=None):
    shape = list(ap.shape)
    d = self.nc.dram_tensor("dbg_" + name, shape, dt or ap.dtype, kind="ExternalOutput").ap()
    self.dma(d, ap)
Em.dbg = _dbg


@contextlib.contextmanager
def _scope(self):
    with contextlib.ExitStack() as es:
        self.dscopes.append(set())
        yield es
        self.barrier()
        for stream in self.dscopes.pop():
            ent = self.dsem.pop(stream)
            self.dfree.append(ent)
            for e in self.ENG:
                self.seen[e].pop("dma:" + stream, None)
Em.scope = _scope


def _rot(self, es, name, shape, dt=F32, n=2):
    pools = getattr(es, "_pools", None)
    if pools is None:
        pools = {}
        es._pools = pools
    ent = pools.get(name)
    if ent is None:
        ent = [[self.sb(name, shape, dt, es) for _ in range(n)], 0]
        pools[name] = ent
    t = ent[0][ent[1] % len(ent[0])]
    ent[1] += 1
    return t
Em.rot = _rot


def _allgather(self, out, in_, groups, stream="cc"):
    reads = [self.key(in_)]
    writes = [self.key(out)]
    if stream not in self.dsem:
        self._new_stream(stream)
    self._deps("pool", reads, writes)
    inst = self.nc.gpsimd.collective_compute("AllGather", op=ALU.bypass, replica_groups=groups, ins=[in_], outs=[out])
    self.dsem[stream][1] += 16
    inst.then_inc(self.dsem[stream][0], 16)
    self.n_inst += 1
    self._commit(("dma:" + stream, self.dsem[stream][1]), reads, writes)
    return inst
Em.allgather = _allgather


def _load_w(self, es, dst, src, kc, ncols, piece=256):
    for c0 in range(0, ncols, piece):
        w = min(piece, ncols - c0)
        stg = self.rot(es, "wstg%d_%d" % (kc, piece), [128, kc, piece], F32, 2)
        self.dma(stg[:, :, 0:w], src[:, c0:c0 + w].rearrange("(c p) n -> p c n", p=128))
        self._wl = getattr(self, "_wl", 0) + 1
        if self._wl % 2:
            self.op("act", "activation", out=dst[:, :, c0:c0 + w], in_=stg[:, :, 0:w], func=AF.Copy)
        else:
            self.op("pool", "tensor_copy", out=dst[:, :, c0:c0 + w], in_=stg[:, :, 0:w])
Em.load_w = _load_w


def peer_keysT(em, es, keys_l, ident_f):
    kf = em.sb("keys_f", [128, 16, 128], F32, es)
    em.dma(kf[:], keys_l.rearrange("h p n d -> n (h p) d"))
    keysT = em.sb("keysT", [128, 16, 128], BF16, es)
    for g in range(4):
        pt = em.ps()
        for k in range(4):
            c = g * 4 + k
            em.mm(pt[:, k * 128:(k + 1) * 128], kf[:, c, :], ident_f[:], transpose=True)
        em.copy(keysT[:, g * 4:(g + 1) * 4, :], pt[:].rearrange("p (c n) -> p c n", c=4), e="act")
    return keysT


def peer_pass(em, nc, es0, DC, hT2, ntp, wq_l, keysT, u_l, v_l, ident_b, acc_out, NI=128, IB=2, dbg=False):
    D = DC * 128
    NT = ntp * 128
    with em.scope() as es:
        stok = [em.sb("stok", [128, 8, 2, 128], F32, es) for _ in range(ntp)]
        diag = [em.sb("diag", [128, 8, 128], BF16, es) for _ in range(ntp)]
        _cm = em.scope()
        es_q = _cm.__enter__()
        qT = em.sb("qT", [128, 16, NT], BF16, es_q)
        wqb = [em.sb("wqb", [128, DC, 256], BF16, es_q) for _ in range(2)]
        for c in range(16):
            wb = wqb[(c // 2) % 2]
            if c % 2 == 0:
                em.load_w(es_q, wb, wq_l[:, c * 128:(c + 2) * 128], DC, 256)
            pq = em.ps()
            for dc in range(DC):
                em.mm(pq[:, 0:NT], wb[:, dc, (c % 2) * 128:(c % 2 + 1) * 128], hT2[:, dc, :], start=(dc == 0), stop=(dc == DC - 1))
            em.copy(qT[:, c, :], pq[:, 0:NT], e="act")
        with em.scope() as es2:
            top = em.sb("top", [128, 2, 16], F32, es2)
            work = em.sb("work", [128, 128], F32, es2)
            cand = em.sb("cand", [128, 16, 16], F32, es2)
            cand2 = em.sb("cand2", [128, 256], F32, es2)
            c24 = em.sb("c24", [128, 24], F32, es2)
            tau = em.sb("tau", [128, 8], F32, es2)
            zs = em.sb("zs", [128, 8], F32, es2)
            ejunk = em.sb("ejunk", [128, 16], F32, es2)
            ntau = em.sb("ntau", [128, 1], F32, es2)
            for tt in range(ntp):
                st = stok[tt]
                for g in range(4):
                    pt = em.ps()
                    for k in range(4):
                        c = g * 4 + k
                        em.mm(pt[:, k * 128:(k + 1) * 128], qT[:, c, tt * 128:(tt + 1) * 128], keysT[:, c, :])
                    em.copy(st[:, 2 * g:2 * g + 2, :, :], pt[:].rearrange("p (h q n) -> p h q n", h=2, q=2), e="act")
                for h in range(8):
                    for p in range(2):
                        src = st[:, h, p, :]
                        em.op("dve", "max", out=top[:, p, 0:8], in_=src)
                        em.op("dve", "match_replace", out=work[:], in_to_replace=top[:, p, 0:8], in_values=src, imm_value=-1e30)
                        em.op("dve", "max", out=top[:, p, 8:16], in_=work[:])
                    em.tt(cand[:], top[:, 0, :].unsqueeze(2).to_broadcast([128, 16, 16]),
                          top[:, 1, :].unsqueeze(1).to_broadcast([128, 16, 16]), ALU.add)
                    cf = cand[:].rearrange("p a b -> p (a b)")
                    em.op("dve", "max", out=c24[:, 0:8], in_=cf)
                    em.op("dve", "match_replace", out=cand2[:], in_to_replace=c24[:, 0:8], in_values=cf, imm_value=-1e30)
                    em.op("dve", "max", out=c24[:, 8:16], in_=cand2[:])
                    em.op("dve", "match_replace", out=cand2[:], in_to_replace=c24[:, 8:16], in_values=cand2[:], imm_value=-1e30)
                    em.op("dve", "max", out=c24[:, 16:24], in_=cand2[:])
                    em.ts(tau[:, h:h + 1], c24[:, 15:16], c24[:, 16:17], ALU.add, 0.5, ALU.mult)
                    em.ts(ntau[:], tau[:, h:h + 1], -1.0, ALU.mult)
                    em.act(ejunk[:], c24[:, 0:16], AF.Exp, bias=ntau[:, 0:1], accum_out=zs[:, h:h + 1])
                em.tt(st[:, :, 0, :], st[:, :, 0, :], tau[:].unsqueeze(2).to_broadcast([128, 8, 128]), ALU.subtract)
                em.op("dve", "reciprocal", out=zs[:], in_=zs[:])
                for h in range(8):
                    em.ts(diag[tt][:, h, :], ident_b[:], zs[:, h:h + 1], ALU.mult)
                if dbg and tt == 0:
                    em.dbg("tau", tau[:]); em.dbg("zs", zs[:]); em.dbg("stok", st[:]); em.dbg("c24", c24[:]); em.dbg("top", top[:])
                    em.dbg("qT", qT[:])
        _cm.__exit__(None, None, None)
        urow = [em.sb("urow", [128, D], BF16, es) for _ in range(2)]
        urowf = [em.sb("urowf", [128, D], F32, es) for _ in range(2)]
        vrowf = [em.sb("vrowf", [128, D], F32, es) for _ in range(2)]
        uT = [em.sb("uT", [128, DC, 128], BF16, es) for _ in range(2)]
        vblk = [[em.sb("vblk", [128, D], BF16, es) for _ in range(IB)] for _ in range(2)]
        gS = [em.sb("gS", [128, NT], BF16, es) for _ in range(2)]
        AT = [[em.sb("AT", [128, NT], BF16, es) for _ in range(IB)] for _ in range(2)]
        Pp = [em.sb("Pp", [128, 8, 128], F32, es) for _ in range(2)]
        Ee = [em.sb("Ee", [128, 8, 128], BF16, es) for _ in range(2)]
        Gg = [em.sb("Gg", [128, 8, 128], BF16, es) for _ in range(2)]
        for a in acc_out:
            em.memset(a[:], 0.0, e="pool")
        nblk = NI // IB
        k2 = 0

        def load(i):
            em.dma(urowf[i % 2][:], u_l[i * 128:(i + 1) * 128, :])
            em.dma(vrowf[i % 2][:], v_l[i * 128:(i + 1) * 128, :])

        load(0)
        for b in range(nblk):
            vb = vblk[b % 2]
            at = AT[b % 2]
            for ii in range(IB):
                i = b * IB + ii
                ur = urow[i % 2]
                ut = uT[i % 2]
                em.op("act", "activation", out=ur[:], in_=urowf[i % 2][:], func=AF.Copy)
                em.op("pool", "tensor_copy", out=vb[ii][:], in_=vrowf[i % 2][:])
                for g0 in range(0, DC, 8):
                    n = min(8, DC - g0)
                    pt = em.ps()
                    ptb = pt[:].bitcast(BF16)
                    for k in range(n):
                        em.mm(ptb[:, k * 128:(k + 1) * 128], ur[:, (g0 + k) * 128:(g0 + k + 1) * 128], ident_b[:], transpose=True)
                    em.copy(ut[:, g0:g0 + n, :], ptb[:, 0:n * 128].rearrange("p (c e) -> p c e", c=n), e="dve")
                if i + 1 < NI:
                    load(i + 1)
                pS = em.ps()
                for dc in range(DC):
                    em.mm(pS[:, 0:NT], ut[:, dc, :], hT2[:, dc, :], start=(dc == 0), stop=(dc == DC - 1))
                gs = gS[i % 2]
                em.act(gs[:], pS[:, 0:NT], AF.Gelu)
                pG = em.ps()
                for tt in range(ntp):
                    st = stok[tt]
                    pp, ee, gg = Pp[k2 % 2], Ee[k2 % 2], Gg[k2 % 2]
                    k2 += 1
                    em.tt(pp[:], st[:, :, 1, :], st[:, :, 0, i:i + 1].to_broadcast([128, 8, 128]), ALU.add, e="pool")
                    em.act(ee[:], pp[:], AF.Exp)
                    em.stt(gg[:], pp[:], 0.0, ee[:], ALU.is_ge, ALU.mult)
                    for h in range(8):
                        em.mm(pG[:, tt * 128:(tt + 1) * 128], gg[:, h, :], diag[tt][:, h, :], start=(h == 0), stop=(h == 7))
                em.tt(at[ii][:], pG[:, 0:NT], gs[:], ALU.mult)
                if dbg and i == 0:
                    em.dbg("gs", gs[:]); em.dbg("at", at[ii][:]); em.dbg("pp", Pp[0][:]); em.dbg("ee", Ee[0][:]); em.dbg("gg", Gg[0][:])
            for tt in range(ntp):
                banks = [em.ps() for _ in range((D + 511) // 512)]
                for cc, bk in enumerate(banks):
                    w = min(512, D - cc * 512)
                    for ii in range(IB):
                        em.mm(bk[:, 0:w], at[ii][:, tt * 128:(tt + 1) * 128], vb[ii][:, cc * 512:cc * 512 + w],
                              start=(ii == 0), stop=(ii == IB - 1))
                for cc, bk in enumerate(banks):
                    w = min(512, D - cc * 512)
                    em.tt(acc_out[tt][:, cc * 512:cc * 512 + w], acc_out[tt][:, cc * 512:cc * 512 + w], bk[:, 0:w], ALU.add,
                          e="dve")
                if dbg and b == 0 and tt == 0:
                    em.dbg("acc0", acc_out[0][:]); em.dbg("vb0", vb[0][:]); em.dbg("vb3", vb[3][:])
                    tmpd = em.sb("tmpd", [128, 512], F32, es); em.copy(tmpd[:], banks[0][:]); em.dbg("bank", tmpd[:])

NEG = -30000.0


def dn_host_consts():
    p = np.arange(128)[:, None]
    f = np.arange(128)[None, :]
    c = np.zeros((128, 6, 128), np.float32)
    c[:, 0] = (p == f)
    c[:, 1] = (p <= f)
    c[:, 2] = (p >= f)
    c[:, 3] = np.where(p > f, 0.0, NEG)
    c[:, 4] = np.where(p < f, 0.0, NEG)
    c[:, 5] = 1.0
    return c


class DnState:
    pass


def dn_setup(em, es, consts_d, NH, aug):
    st = DnState()
    st.NH = NH
    st.W = 256 if aug else 128
    st.cst = em.sb("dncst", [128, 6, 128], F32, es)
    em.dma(st.cst[:], consts_d)
    st.identb = em.sb("dnidb", [128, 128], BF16, es)
    em.copy(st.identb[:], st.cst[:, 0, :])
    st.negT = em.sb("dnnegT", [128, 2, 128], F32, es)
    em.ts(st.negT[:], st.cst[:, 1:3, :], -1.0, ALU.add, -NEG, ALU.mult)
    st.S = [[em.sb("S", [128, st.W], F32, es) for _ in range(NH)] for _ in range(2)]
    st.Sb = [[em.sb("Sb", [128, st.W], BF16, es) for _ in range(NH)] for _ in range(2)]
    return st


def dn_chunk(em, es, st, d, kT, qT, ktok, vtok, beta, g, rep, want_o, o_out, aug=False, dbg=None, stage=99):
    NH = st.NH
    W = st.W
    ident = st.cst[:, 0, :]
    ones = st.cst[:, 5, :]
    Mincl = st.cst[:, 1 + d, :]
    negS = st.cst[:, 3 + d, :]
    negT = st.negT[:, d, :]
    pc = em.ps()
    em.mm(pc[:, 0:NH], Mincl, g[:, :])
    em.mm(pc[:, 128:128 + NH], ones, g[:, :])
    gc = em.rot(es, "gc", [128, 6, NH], F32, 2)
    em.copy(gc[:, 0, :], pc[:, 0:NH])
    em.ts(gc[:, 1, :], pc[:, 0:NH], -1.0, ALU.mult)
    em.act(gc[:, 2, :], pc[:, 0:NH], AF.Exp)
    em.tt(gc[:, 2, :], gc[:, 2, :], beta[:, :], ALU.mult)
    em.tt(gc[:, 5, :], pc[:, 128:128 + NH], gc[:, 0, :], ALU.subtract)
    em.act(gc[:, 3, :], gc[:, 5, :], AF.Exp)
    em.act(gc[:, 4, :], pc[:, 128:128 + NH], AF.Exp)
    if stage < 1:
        return
    nhk = NH // rep
    for hk in range(nhk):
        pk = em.ps()
        em.mm(pk[:, 0:128], kT[:, hk, :], kT[:, hk, :])
        em.mm(pk[:, 128:256], kT[:, hk, :], qT[:, hk, :])
        kkq = em.rot(es, "kkq", [128, 256], F32, 2)
        em.copy(kkq[:], pk[:, 0:256], e="act")
        for r in range(rep):
            h = hk * rep + r
            S, Sb = st.S[d][h], st.Sb[d][h]
            gM = em.rot(es, "gM", [128, 128], F32, 2)
            em.ts(gM[:], Mincl, g[:, h:h + 1], ALU.mult, e="pool")
            pg = em.ps()
            em.mm(pg[:, 0:128], ones, gM[:])
            t1 = em.rot(es, "t1", [128, 128], F32, 2)
            em.stt(t1[:], pg[:, 0:128], -1.0, negS, ALU.mult, ALU.add)
            em.act(t1[:], t1[:], AF.Exp, bias=gc[:, 0, h:h + 1])
            A = em.rot(es, "A", [128, 128], F32, 2)
            em.stt(A[:], t1[:], beta[:, h:h + 1], kkq[:, 0:128], ALU.mult, ALU.mult)
            t2 = em.rot(es, "t2", [128, 128], F32, 2)
            em.tt(t2[:], pg[:, 0:128], negT, ALU.add)
            em.act(t2[:], t2[:], AF.Exp, bias=gc[:, 1, h:h + 1])
            if want_o:
                qkT = em.rot(es, "qkT", [128, 128], BF16, 2)
                em.tt(qkT[:], t2[:], kkq[:, 128:256], ALU.mult, e="pool")
                eg = em.rot(es, "eg", [128, 128], F32, 2)
                em.act(eg[:], pg[:, 0:128], AF.Exp)
                qgT = em.rot(es, "qgT", [128, 128], BF16, 2)
                em.tt(qgT[:], qT[:, hk, :], eg[:], ALU.mult, e="pool")
            if stage < 2:
                continue
            pa = em.ps()
            em.mm(pa[:, 0:128], A[:], ident, transpose=True)
            B = em.rot(es, "B", [128, 128], F32, 3)
            BT = em.rot(es, "BT", [128, 128], F32, 3)
            P = em.rot(es, "P", [128, 128], F32, 3)
            em.ts(B[:], A[:], -1.0, ALU.mult, e="pool")
            em.ts(BT[:], pa[:, 0:128], -1.0, ALU.mult)
            em.tt(P[:], BT[:], ident, ALU.add, e="pool")
            if stage < 2.2:
                continue
            for lvl in range(1, 7 if stage >= 2.6 else 2):
                last = lvl == 6
                if stage < 2.4 and lvl >= 1:
                    pb = em.ps()
                    em.mm(pb[:, 0:128], BT[:], B[:])
                    continue
                pb = em.ps()
                em.mm(pb[:, 0:128], BT[:], B[:])
                if not last:
                    em.mm(pb[:, 128:256], B[:], BT[:])
                if stage < 2.42:
                    continue
                B2 = em.rot(es, "B", [128, 128], F32, 3)
                em.copy(B2[:], pb[:, 0:128], e="act")
                if stage < 2.44:
                    B = B2
                    continue
                if not last:
                    BT2 = em.rot(es, "BT", [128, 128], F32, 3)
                    em.ts(BT2[:], pb[:, 128:256], 1.0, ALU.mult) if "dve" == "dve" else em.copy(BT2[:], pb[:, 128:256], e="act")
                    BT = BT2
                B = B2
                if stage < 2.5:
                    continue
                pp = em.ps()
                em.mm(pp[:, 0:128], B[:], P[:])
                P2 = em.rot(es, "P", [128, 128], F32, 3)
                em.tt(P2[:], P[:], pp[:, 0:128], ALU.add)
                P = P2
            if stage < 3:
                continue
            Pb = em.rot(es, "Pb", [128, 128], BF16, 2)
            em.copy(Pb[:], P[:], e="act")
            bv = em.rot(es, "bv", [128, 128], BF16, 2)
            em.ts(bv[:], vtok[:, h, :], beta[:, h:h + 1], ALU.mult, e="pool")
            bk = em.rot(es, "bk", [128, 128], BF16, 2)
            em.ts(bk[:], ktok[:, hk, :], gc[:, 2, h:h + 1], ALU.mult, e="pool")
            kd = em.rot(es, "kd", [128, 128], BF16, 2)
            em.ts(kd[:], ktok[:, hk, :], gc[:, 3, h:h + 1], ALU.mult, e="pool")
            pw = em.ps()
            em.mm(pw[:, 0:128], bk[:], Pb[:])
            nwkT = em.rot(es, "nwkT", [128, 128], BF16, 2)
            em.ts(nwkT[:], pw[:, 0:128], -1.0, ALU.mult)
            pwv = em.ps()
            em.mm(pwv[:, 0:128], Pb[:], bv[:], start=True, stop=False)
            em.mm(pwv[:, 0:128], nwkT[:], Sb[:, 0:128], start=False, stop=True)
            if aug:
                em.mm(pwv[:, 128:256], nwkT[:], Sb[:, 128:256], start=True, stop=True)
            wb = em.rot(es, "wb", [128, W], BF16, 2)
            em.copy(wb[:], pwv[:, 0:W], e="act")
            if want_o:
                po = em.ps()
                em.mm(po[:, 0:128], qgT[:], Sb[:, 0:128], start=True, stop=False)
                em.mm(po[:, 0:128], qkT[:], wb[:, 0:128], start=False, stop=True)
                em.copy(o_out[:, h, :], po[:, 0:128], e="act")
            pS = em.ps()
            em.mm(pS[:, 0:W], kd[:], wb[:])
            em.stt(S[:], S[:], gc[:, 4, h:h + 1], pS[:, 0:W], ALU.mult, ALU.add)
            em.copy(Sb[:], S[:], e="act")
            if dbg is not None and h == 0:
                dbg(dict(A=A, P=P, t2=t2, wb=wb, kkq=kkq, gc=gc))

D = 2048
DC = 16
LP = 256
NPS = 2
LS = 4096
LTOT = NPS * LP + LS
DN_QKV = 8192
EPS = 1e-6


def dram(nc, name, shape, dt):
    return nc.dram_tensor(name, list(shape), dt, kind="Internal").ap()


class Ctx:
    pass


def bcast_row(ap_row, n=128):
    a = ap_row.partition_broadcast(n)
    if len(a.shape) == 3:
        a = a.rearrange("p o d -> p (o d)")
    return a


def load_mod(em, es, cx, layer, variant, k, name):
    t = em.rot(es, name, [128, D], F32, 1)
    em.dma(t[:], bcast_row(cx.mod[layer, variant:variant + 1, k * D:(k + 1) * D]))
    return t


def make_gm(em, es, cx, layer, variant, which, name):
    sh = load_mod(em, es, cx, layer, variant, 3 * which + 0, name + "sh")
    sc = load_mod(em, es, cx, layer, variant, 3 * which + 1, name + "sc")
    g = em.rot(es, name + "g", [128, D], F32, 1)
    gsrc = (cx.norm1_g if which == 0 else cx.norm2_g)[layer:layer + 1, :]
    em.dma(g[:], bcast_row(gsrc))
    em.stt(sc[:], sc[:], 1.0, g[:], ALU.add, ALU.mult)
    return sc, sh


def norm_mod_T(em, es, cx, xt, gm, sh, hT, col0):
    junk = em.rot(es, "nm_junk", [128, D], BF16, 1)
    ss = em.rot(es, "nm_ss", [128, 1], F32, 2)
    em.act(junk[:], xt[:], AF.Square, accum_out=ss[:])
    rs = em.rot(es, "nm_rs", [128, 1], F32, 2)
    em.ts(rs[:], ss[:], 1.0 / D, ALU.mult, EPS, ALU.add)
    em.act(rs[:], rs[:], AF.Sqrt)
    em.op("dve", "reciprocal", out=rs[:], in_=rs[:])
    t = em.rot(es, "nm_t", [128, D], F32, 1)
    em.stt(t[:], xt[:], rs[:, 0:1], gm[:], ALU.mult, ALU.mult)
    hb = em.rot(es, "nm_hb", [128, D], BF16, 2)
    em.tt(hb[:], t[:], sh[:], ALU.add, e="pool")
    for g0 in range(0, DC, 8):
        pt = em.ps()
        ptb = pt[:].bitcast(BF16)
        for k in range(8):
            em.mm(ptb[:, k * 128:(k + 1) * 128], hb[:, (g0 + k) * 128:(g0 + k + 1) * 128], cx.identb[:], transpose=True)
        em.copy(hT[:, g0:g0 + 8, col0:col0 + 128], ptb[:].rearrange("p (c t) -> p c t", c=8), e="act")


def x_src(cx, layer_in, tok0):
    if layer_in == 0:
        if tok0 < NPS * LP:
            return cx.xp[tok0:tok0 + 128, :]
        return cx.xs[tok0 - NPS * LP:tok0 - NPS * LP + 128, :]
    return cx.xres[layer_in - 1][tok0:tok0 + 128, :]


def phase_adaln(em, cx):
    nc = cx.nc
    with em.scope() as es:
        cv = em.sb("cv", [128, DC, 2], F32, es)
        with cx.nc.allow_non_contiguous_dma("tiny transposed load of conditioning vectors"):
            for v in range(2):
                em.dma(cv[:, :, v], cx.cvec[v, :].rearrange("(c p) -> p c", p=128))
        em.act(cv[:], cv[:], AF.Silu)
        for l in range(2):
            for cc in range(24):
                w = em.rot(es, "adaw", [128, DC, 512], F32, 2)
                em.dma(w[:], cx.ada_w[l, :, cc * 512:(cc + 1) * 512].rearrange("(c p) n -> p c n", p=128))
                b = em.rot(es, "adab", [2, 512], F32, 2)
                em.dma(b[:], bcast_row(cx.ada_b[l:l + 1, cc * 512:(cc + 1) * 512], 2))
                pm = em.ps()
                for dc in range(DC):
                    em.mm(pm[0:2, :], cv[:, dc, :], w[:, dc, :], start=(dc == 0), stop=(dc == DC - 1))
                m = em.rot(es, "adam", [2, 512], F32, 2)
                em.tt(m[:], pm[0:2, :], b[:], ALU.add)
                em.dma(cx.mod[l, :, cc * 512:(cc + 1) * 512], m[:])


def phase_dn_proj(em, cx):
    groups = [(0, NPS * LP, 0)] + [(NPS * LP + i * 512, 512, 1) for i in range(LS // 512)]
    with em.scope() as es:
        ea = em.sb("ea", [128, 64], F32, es)
        dtb = em.sb("dtb", [128, 64], F32, es)
        em.dma(ea[:], bcast_row(cx.dn_a_log.rearrange("o d h -> o (d h)")))
        em.dma(dtb[:], bcast_row(cx.dn_dt_bias.rearrange("o d h -> o (d h)")))
        em.act(ea[:], ea[:], AF.Exp)
        hT = em.sb("hT", [128, DC, 512], BF16, es)
        cur_var = None
        for (t0, n, var) in groups:
            if var != cur_var:
                gm, sh = make_gm(em, es, cx, 0, var, 0, "n1")
                cur_var = var
            for tt in range(n // 128):
                xt = em.rot(es, "xt", [128, D], F32, 2)
                em.dma(xt[:], x_src(cx, 0, t0 + tt * 128))
                norm_mod_T(em, es, cx, xt, gm, sh, hT, tt * 128)
            for c in range(64):
                if c % 2 == 0:
                    wb = em.rot(es, "winb", [128, DC, 256], BF16, 2)
                    em.load_w(es, wb, cx.dn_w_in[0, :, c * 128:(c + 2) * 128], DC, 256)
                pp = em.ps()
                for dc in range(DC):
                    em.mm(pp[:, 0:n], wb[:, dc, (c % 2) * 128:(c % 2 + 1) * 128], hT[:, dc, 0:n], start=(dc == 0), stop=(dc == DC - 1))
                pj = em.rot(es, "pj", [128, 512], F32, 3)
                em.copy(pj[:, 0:n], pp[:, 0:n], e="act")
                em.dma(cx.projT[c, :, t0:t0 + n], pj[:, 0:n])
            for cc in range(8):
                wz = em.rot(es, "wz", [128, DC, 512], BF16, 2)
                em.load_w(es, wz, cx.dn_w_in[0, :, DN_QKV + cc * 512:DN_QKV + (cc + 1) * 512], DC, 512)
                for tt in range(n // 128):
                    pz = em.ps()
                    for dc in range(DC):
                        em.mm(pz[:], hT[:, dc, tt * 128:(tt + 1) * 128], wz[:, dc, :], start=(dc == 0), stop=(dc == DC - 1))
                    zt = em.rot(es, "zt", [128, 512], BF16, 3)
                    em.act(zt[:], pz[:], AF.Silu)
                    em.dma(cx.z[t0 + tt * 128:t0 + (tt + 1) * 128, cc * 512:(cc + 1) * 512], zt[:])
            wba = em.rot(es, "wba", [128, DC, 128], BF16, 1)
            em.load_w(es, wba, cx.dn_w_in[0, :, DN_QKV + 4096:DN_QKV + 4096 + 128], DC, 128)
            for tt in range(n // 128):
                pb = em.ps()
                for dc in range(DC):
                    em.mm(pb[:, 0:128], hT[:, dc, tt * 128:(tt + 1) * 128], wba[:, dc, :], start=(dc == 0), stop=(dc == DC - 1))
                bg = em.rot(es, "bgt", [128, 128], F32, 2)
                em.act(bg[:, 0:64], pb[:, 0:64], AF.Sigmoid)
                tmp = em.rot(es, "bgtmp", [128, 64], F32, 2)
                em.tt(tmp[:], pb[:, 64:128], dtb[:], ALU.add)
                em.act(tmp[:], tmp[:], AF.Exp)
                em.act(tmp[:], tmp[:], AF.Ln, bias=1.0)
                em.stt(bg[:, 64:128], tmp[:], -1.0, ea[:], ALU.mult, ALU.mult)
                em.dma(cx.bg[t0 + tt * 128:t0 + (tt + 1) * 128, :], bg[:])


def phase_dn_conv(em, cx):
    seqs = [(i * LP, LP) for i in range(NPS)] + [(NPS * LP, LS)]
    with em.scope() as es:
        cw = em.sb("convw", [128, 5, 64], F32, es)
        with cx.nc.allow_non_contiguous_dma("small transposed conv weight load"):
            for k in range(5):
                em.dma(cw[:, k, :], cx.dn_conv_w[0, k, :].rearrange("(c p) -> p c", p=128))
        onesb = em.sb("onesb", [128, 128], BF16, es)
        em.memset(onesb[:], 1.0)
        for (t0, L) in seqs:
            for c in range(64):
                xin = em.rot(es, "cv_in", [128, LS + 4], F32, 2)
                em.memset(xin[:, 0:2], 0.0, e="pool")
                em.memset(xin[:, L + 2:L + 4], 0.0, e="pool")
                em.dma(xin[:, 2:L + 2], cx.projT[c, :, t0:t0 + L])
                acc = em.rot(es, "cv_acc", [128, LS], F32, 2)
                em.ts(acc[:, 0:L], xin[:, 0:L], cw[:, 0, c:c + 1], ALU.mult)
                for k in range(1, 5):
                    em.stt(acc[:, 0:L], xin[:, k:k + L], cw[:, k, c:c + 1], acc[:, 0:L], ALU.mult, ALU.add,
                           e=("dve" if k % 2 else "dve"))
                so = em.rot(es, "cv_so", [128, LS], BF16, 2)
                em.act(so[:, 0:L], acc[:, 0:L], AF.Silu)
                if c < 32:
                    sq = em.rot(es, "cv_sq", [128, LS], BF16, 1)
                    em.tt(sq[:, 0:L], so[:, 0:L], so[:, 0:L], ALU.mult, e="pool")
                    nrm = em.rot(es, "cv_nrm", [128, LS], BF16, 2)
                    for b0 in range(0, L, 512):
                        w = min(512, L - b0)
                        pn = em.ps()
                        em.mm(pn[:, 0:w], onesb[:], sq[:, b0:b0 + w])
                        rn = em.rot(es, "cv_rn", [128, 512], F32, 2)
                        em.ts(rn[:, 0:w], pn[:, 0:w], EPS, ALU.add)
                        em.act(rn[:, 0:w], rn[:, 0:w], AF.Sqrt)
                        em.op("dve", "reciprocal", out=rn[:, 0:w], in_=rn[:, 0:w])
                        if c < 16:
                            em.stt(nrm[:, b0:b0 + w], so[:, b0:b0 + w], 128 ** -0.5, rn[:, 0:w], ALU.mult, ALU.mult)
                        else:
                            em.tt(nrm[:, b0:b0 + w], so[:, b0:b0 + w], rn[:, 0:w], ALU.mult)
                    if c < 16:
                        em.dma(cx.qT[c, :, t0:t0 + L], nrm[:, 0:L])
                    else:
                        em.dma(cx.kT[c - 16, :, t0:t0 + L], nrm[:, 0:L])
                    src = nrm
                else:
                    src = so
                if c >= 16:
                    for b0 in range(0, L, 1024):
                        nt = min(8, (L - b0) // 128)
                        pt = em.ps()
                        ptb = pt[:].bitcast(BF16)
                        for k in range(nt):
                            em.mm(ptb[:, k * 128:(k + 1) * 128], src[:, b0 + k * 128:b0 + (k + 1) * 128], cx.identb[:], transpose=True)
                        tk = em.rot(es, "cv_tk", [128, 8, 128], BF16, 2)
                        em.copy(tk[:, 0:nt, :], ptb[:, 0:nt * 128].rearrange("p (t d) -> p t d", t=nt), e="act")
                        if c < 32:
                            dst = cx.ktok[t0 + b0:t0 + b0 + nt * 128, c - 16, :]
                        else:
                            dst = cx.vtok[t0 + b0:t0 + b0 + nt * 128, c - 32, :]
                        em.dma(dst.rearrange("(t p) d -> p t d", p=128), tk[:, 0:nt, :])


def phase_dn_scan(em, cx):
    seqs = [(i * LP, LP, i) for i in range(NPS)] + [(NPS * LP, LS, -1)]
    with em.scope() as es:
        st = dn_setup(em, es, cx.dncst, 32, False)
        for (t0, L, pi) in seqs:
            nch = L // 128
            for d in range(2):
                for h in range(32):
                    if pi >= 0:
                        em.memset(st.S[d][h][:], 0.0, e="pool")
                        em.memset(st.Sb[d][h][:], 0.0, e="pool")
                    else:
                        em.dma(st.S[d][h][:], cx.s0[d, h], stream="s0ld")
                        em.copy(st.Sb[d][h][:], st.S[d][h][:], e="act")
                order = range(nch) if d == 0 else range(nch - 1, -1, -1)
                for c in order:
                    a = t0 + c * 128
                    kT = em.rot(es, "s_kT", [128, 16, 128], BF16, 2)
                    em.dma(kT[:], cx.kT[:, :, a:a + 128].rearrange("h p t -> p h t"))
                    qT = em.rot(es, "s_qT", [128, 16, 128], BF16, 2)
                    em.dma(qT[:], cx.qT[:, :, a:a + 128].rearrange("h p t -> p h t"))
                    kt = em.rot(es, "s_kt", [128, 16, 128], BF16, 2)
                    em.dma(kt[:], cx.ktok[a:a + 128])
                    vt = em.rot(es, "s_vt", [128, 32, 128], BF16, 2)
                    em.dma(vt[:], cx.vtok[a:a + 128])
                    bgt = em.rot(es, "s_bg", [128, 128], F32, 2)
                    em.dma(bgt[:], cx.bg[a:a + 128, :])
                    oo = em.rot(es, "s_oo", [128, 32, 128], F32, 2)
                    dn_chunk(em, es, st, d, kT, qT, kt, vt, bgt[:, d * 32:(d + 1) * 32], bgt[:, 64 + d * 32:64 + (d + 1) * 32], 2, True, oo)
                    em.dma(cx.odn[d, a:a + 128, :], oo[:].rearrange("p h d -> p (h d)"))
                if pi >= 0:
                    for h in range(32):
                        em.dma(cx.nsd[pi, d, h], st.S[d][h][:], stream="nsdst")


def phase_post(em, cx, layer):
    groups = [(0, NPS * LP, 0)] + [(NPS * LP + i * 512, 512, 1) for i in range(LS // 512)]
    NCH = 32 if layer == 0 else 16
    w_o = cx.dn_w_o[0] if layer == 0 else cx.na_w_o[0]
    with em.scope() as es:
        keysT = peer_keysT(em, es, cx.peer_keys[layer], cx.identf)
        if layer == 0:
            gdn = em.sb("gdn", [128, 128], F32, es)
            em.dma(gdn[:], bcast_row(cx.dn_norm_g[0:1, :]))
        if layer == 1:
            fg = em.sb("fg", [128, D], F32, es)
            em.dma(fg[:], bcast_row(cx.final_g.rearrange("(o d) -> o d", o=1)))
        h2T = em.sb("h2T", [128, DC, 512], BF16, es)
        for (t0, n, var) in groups:
            ntp = n // 128
            with em.scope() as eg:
                x1 = [em.sb("x1", [128, D], F32, eg) for _ in range(ntp)]
                for tt in range(ntp):
                    em.dma(x1[tt][:], x_src(cx, layer, t0 + tt * 128))
                with em.scope() as eb:
                    g1 = load_mod(em, eb, cx, layer, var, 2, "g1")
                    ogT = em.sb("ogT", [128, NCH, 512], BF16, eb)
                    if layer == 0:
                        with em.scope() as ea:
                            for tt in range(ntp):
                                a = t0 + tt * 128
                                of = em.rot(ea, "of", [128, 32, 128], F32, 1)
                                ob = em.rot(ea, "ob", [128, 32, 128], F32, 1)
                                em.dma(of[:], cx.odn[0, a:a + 128, :].rearrange("p (h d) -> p h d", h=32))
                                em.dma(ob[:], cx.odn[1, a:a + 128, :].rearrange("p (h d) -> p h d", h=32))
                                zt = em.rot(ea, "zg", [128, 32, 128], BF16, 1)
                                em.dma(zt[:], cx.z[a:a + 128, :].rearrange("p (h d) -> p h d", h=32))
                                em.tt(of[:], of[:], ob[:], ALU.add, e="pool")
                                em.tt(ob[:], of[:], of[:], ALU.mult)
                                ms = em.rot(ea, "ms", [128, 32], F32, 2)
                                em.op("dve", "tensor_reduce", out=ms[:], in_=ob[:], op=ALU.add, axis=AX.X)
                                em.ts(ms[:], ms[:], 1.0 / 128, ALU.mult, EPS, ALU.add)
                                em.act(ms[:], ms[:], AF.Sqrt)
                                em.op("dve", "reciprocal", out=ms[:], in_=ms[:])
                                em.tt(of[:], of[:], ms[:].unsqueeze(2).to_broadcast([128, 32, 128]), ALU.mult)
                                em.tt(of[:], of[:], gdn[:].unsqueeze(1).to_broadcast([128, 32, 128]), ALU.mult, e="pool")
                                og = em.rot(ea, "og", [128, 32 * 128], BF16, 2)
                                em.tt(og[:].rearrange("p (h d) -> p h d", h=32), of[:], zt[:], ALU.mult)
                                for g0 in range(0, 32, 8):
                                    pt = em.ps()
                                    ptb = pt[:].bitcast(BF16)
                                    for k in range(8):
                                        em.mm(ptb[:, k * 128:(k + 1) * 128], og[:, (g0 + k) * 128:(g0 + k + 1) * 128], cx.identb[:], transpose=True)
                                    em.copy(ogT[:, g0:g0 + 8, tt * 128:(tt + 1) * 128], ptb[:].rearrange("p (c t) -> p c t", c=8), e="act")
                    else:
                        em.dma(ogT[:, :, 0:n], cx.oT[:, :, t0:t0 + n].rearrange("h p t -> p h t"))
                    for cc in range(4):
                        wo = em.rot(eb, "wo", [128, NCH, 512], BF16, 1)
                        for hf in range(2):
                            hs = slice(hf * NCH // 2, (hf + 1) * NCH // 2)
                            em.load_w(eb, wo[:, hs, :], w_o[hf * NCH * 64:(hf + 1) * NCH * 64, cc * 512:(cc + 1) * 512], NCH // 2, 512)
                        for tt in range(ntp):
                            po = em.ps()
                            for ch in range(NCH):
                                em.mm(po[:], ogT[:, ch, tt * 128:(tt + 1) * 128], wo[:, ch, :], start=(ch == 0), stop=(ch == NCH - 1))
                            tmp = em.rot(eb, "potmp", [128, 512], F32, 2)
                            em.tt(tmp[:], po[:], g1[:, cc * 512:(cc + 1) * 512], ALU.mult)
                            em.tt(x1[tt][:, cc * 512:(cc + 1) * 512], x1[tt][:, cc * 512:(cc + 1) * 512], tmp[:], ALU.add, e="pool")
                gm2, sh2 = make_gm(em, eg, cx, layer, var, 1, "n2")
                for tt in range(ntp):
                    norm_mod_T(em, eg, cx, x1[tt], gm2, sh2, h2T, tt * 128)
                    em.dma(cx.xmid[t0 + tt * 128:t0 + (tt + 1) * 128, :], x1[tt][:])
            with em.scope() as ep:
                acc = [em.sb("pacc", [128, D], F32, ep) for _ in range(ntp)]
                peer_pass(em, cx.nc, ep, DC, h2T, ntp, cx.peer_w_q[layer], keysT, cx.peer_u[layer], cx.peer_v[layer], cx.identb, acc)
                g2 = load_mod(em, ep, cx, layer, var, 5, "g2")
                for tt in range(ntp):
                    a = t0 + tt * 128
                    xr = em.rot(ep, "xr", [128, D], F32, 2)
                    em.dma(xr[:], cx.xmid[a:a + 128, :])
                    em.tt(acc[tt][:], acc[tt][:], g2[:], ALU.mult, e="pool")
                    em.tt(xr[:], xr[:], acc[tt][:], ALU.add)
                    if layer == 0:
                        em.dma(cx.xres[0][a:a + 128, :], xr[:])
                    else:
                        junk = em.rot(ep, "fjunk", [128, D], BF16, 1)
                        ss = em.rot(ep, "fss", [128, 1], F32, 2)
                        em.act(junk[:], xr[:], AF.Square, accum_out=ss[:])
                        em.ts(ss[:], ss[:], 1.0 / D, ALU.mult, EPS, ALU.add)
                        em.act(ss[:], ss[:], AF.Sqrt)
                        em.op("dve", "reciprocal", out=ss[:], in_=ss[:])
                        em.stt(acc[tt][:], xr[:], ss[:, 0:1], fg[:], ALU.mult, ALU.mult)
                        if a < NPS * LP:
                            em.dma(cx.yp[a:a + 128, :], acc[tt][:])
                        else:
                            em.dma(cx.ys[a - NPS * LP:a - NPS * LP + 128, :], acc[tt][:])


NA_SCALE = 128 ** -0.5


def attend(em, es, cx, chunks, qT_ap, nq, oT_dst, etab_ap=None):
    nchk = len(chunks)
    pS = em.ps()
    for i, (kt, v) in enumerate(chunks):
        em.mm(pS[:, i * nq:(i + 1) * nq], kt, qT_ap)
    E = em.rot(es, "at_E", [128, 512], BF16, 3)
    em.act(E[:, 0:nchk * nq], pS[:, 0:nchk * nq], AF.Exp, scale=NA_SCALE)
    if etab_ap is not None:
        nl = etab_ap.shape[1]
        v3 = E[:, 0:nl * nq].rearrange("p (m q) -> p m q", m=nl)
        em.tt(v3, v3, etab_ap, ALU.mult)
    pN = em.ps()
    for i, (kt, v) in enumerate(chunks):
        em.mm(pN[:, 0:nq], v, E[:, i * nq:(i + 1) * nq], start=(i == 0), stop=(i == nchk - 1))
    for i in range(nchk):
        em.mm(pN[:, 256:256 + nq], cx.onesb[:], E[:, i * nq:(i + 1) * nq], start=(i == 0), stop=(i == nchk - 1))
    rd = em.rot(es, "at_rd", [128, 256], F32, 3)
    em.op("dve", "reciprocal", out=rd[:, 0:nq], in_=pN[:, 256:256 + nq])
    em.tt(oT_dst, pN[:, 0:nq], rd[:, 0:nq], ALU.mult)


def phase_na(em, cx):
    groups = [(0, NPS * LP, 0)] + [(NPS * LP + i * 512, 512, 1) for i in range(LS // 512)]
    wq = cx.na_w_qkv[0]
    with em.scope() as es:
        hT = em.sb("hT", [128, DC, 512], BF16, es)
        for (t0, n, var) in groups:
            with em.scope() as eg:
                gm, sh = make_gm(em, eg, cx, 1, var, 0, "n1")
                for tt in range(n // 128):
                    xt = em.rot(eg, "xt", [128, D], F32, 2)
                    em.dma(xt[:], x_src(cx, 1, t0 + tt * 128))
                    norm_mod_T(em, eg, cx, xt, gm, sh, hT, tt * 128)
                for c in range(32):
                    if c % 2 == 0:
                        wb = em.rot(eg, "wqkb", [128, DC, 256], BF16, 2)
                        em.load_w(eg, wb, wq[:, c * 128:(c + 2) * 128], DC, 256)
                    pp = em.ps()
                    for dc in range(DC):
                        em.mm(pp[:, 0:n], wb[:, dc, (c % 2) * 128:(c % 2 + 1) * 128], hT[:, dc, 0:n], start=(dc == 0), stop=(dc == DC - 1))
                    pj = em.rot(eg, "pjn", [128, 512], BF16, 3)
                    em.copy(pj[:, 0:n], pp[:, 0:n], e="act")
                    dst = cx.qnT if c < 16 else cx.knT
                    em.dma(dst[c % 16, :, t0:t0 + n], pj[:, 0:n])
                for which in ([2, 1] if var == 0 else [2]):
                    for cc in range(4):
                        wv = em.rot(eg, "wvb", [128, DC, 512], BF16, 2)
                        em.load_w(eg, wv, wq[:, which * D + cc * 512:which * D + (cc + 1) * 512], DC, 512)
                        for tt in range(n // 128):
                            a = t0 + tt * 128
                            pv = em.ps()
                            for dc in range(DC):
                                em.mm(pv[:], hT[:, dc, tt * 128:(tt + 1) * 128], wv[:, dc, :], start=(dc == 0), stop=(dc == DC - 1))
                            if which == 2:
                                vb = em.rot(eg, "vnb", [128, 512], BF16, 3)
                                em.copy(vb[:], pv[:], e="act")
                                em.dma(cx.vn[a:a + 128, cc * 512:(cc + 1) * 512], vb[:])
                            if var == 0:
                                vf = em.rot(eg, "vnf", [128, 512], F32, 3)
                                em.ts(vf[:], pv[:], 1.0, ALU.mult)
                                dst = cx.ncv if which == 2 else cx.nck
                                em.dma(dst[a:a + 128, cc * 512:(cc + 1) * 512], vf[:])
    with em.scope() as es:
        for s in range(NPS):
            t0 = s * LP
            for h in range(16):
                kt = em.rot(es, "c_kt", [128, LP], BF16, 2)
                em.dma(kt[:], cx.knT[h, :, t0:t0 + LP])
                qt = em.rot(es, "c_qt", [128, LP], BF16, 2)
                em.dma(qt[:], cx.qnT[h, :, t0:t0 + LP])
                vv = em.rot(es, "c_v", [128, LP // 128, 128], BF16, 2)
                em.dma(vv[:], cx.vn[t0:t0 + LP, h * 128:(h + 1) * 128].rearrange("(t p) d -> p t d", p=128))
                ot = em.rot(es, "c_ot", [128, LP], BF16, 2)
                chunks = [(kt[:, kc * 128:(kc + 1) * 128], vv[:, kc, :]) for kc in range(LP // 128)]
                attend(em, es, cx, chunks, qt[:, :], LP, ot[:, :])
                em.dma(cx.oT[h, :, t0:t0 + LP], ot[:])
    with em.scope() as es:
        t0 = NPS * LP
        kcT = em.sb("kcT", [128, 16, 256], BF16, es)
        vc = em.sb("vc", [128, 2, 16, 128], BF16, es)
        for tt in range(2):
            ck = em.rot(es, "ckf", [128, 16, 128], F32, 2)
            em.dma(ck[:], cx.ck[tt * 128:(tt + 1) * 128])
            em.dma(vc[:, tt], cx.cv[tt * 128:(tt + 1) * 128], q="pool")
            for g0 in range(0, 16, 4):
                pt = em.ps()
                for k in range(4):
                    em.mm(pt[:, k * 128:(k + 1) * 128], ck[:, g0 + k, :], cx.identf[:], transpose=True)
                em.copy(kcT[:, g0:g0 + 4, tt * 128:(tt + 1) * 128], pt[:].rearrange("p (h t) -> p h t", h=4), e="act")
        okm = em.sb("okm", [128, 64], F32, es)
        em.dma(okm[:], cx.okm)
        for h in range(16):
            rp = em.rot(es, "rpx", [128, 14, 64], F32, 2)
            em.dma(rp[:], cx.rpbx[:, h])
            em.act(rp[:], rp[:], AF.Exp)
            etab = em.rot(es, "etab", [128, 14, 64], BF16, 2)
            em.tt(etab[:], rp[:], okm[:].unsqueeze(1).to_broadcast([128, 14, 64]), ALU.mult)
            kt = em.rot(es, "l_kt", [128, LS], BF16, 2)
            em.dma(kt[:], cx.knT[h, :, t0:t0 + LS])
            qt = em.rot(es, "l_qt", [128, LS], BF16, 2)
            em.dma(qt[:], cx.qnT[h, :, t0:t0 + LS])
            ve = em.rot(es, "l_ve", [128, 32, 128], BF16, 2)
            em.dma(ve[:], cx.vn[t0:t0 + LS, h * 128:(h + 1) * 128].rearrange("(t p) d -> p t d", p=128))
            vo = em.rot(es, "l_vo", [128, 31, 128], BF16, 2)
            em.dma(vo[:], cx.vn[t0 + 64:t0 + LS - 64, h * 128:(h + 1) * 128].rearrange("(t p) d -> p t d", p=128))
            ot = em.rot(es, "l_ot", [128, LS], BF16, 2)
            for r in range(64):
                r0 = min(max(r - 4, 0), 56)
                chunks = []
                for m in range(4):
                    a = (r0 + 2 * m) * 64
                    vsrc = ve[:, a // 128, :] if a % 128 == 0 else vo[:, (a - 64) // 128, :]
                    chunks.append((kt[:, a:a + 128], vsrc))
                for kc in range(2):
                    chunks.append((kcT[:, h, kc * 128:(kc + 1) * 128], vc[:, kc, h, :]))
                base = r0 - r + 7
                attend(em, es, cx, chunks, qt[:, r * 64:(r + 1) * 64], 64, ot[:, r * 64:(r + 1) * 64],
                       etab_ap=etab[:, base:base + 7:2, :])
            em.dma(cx.oT[h, :, t0:t0 + LS], ot[:])


_IN_SPECS = [
    ("xp", [NPS * LP, D], F32), ("xs", [LS, D], F32), ("cvec", [2, D], F32),
    ("s0", [2, 32, 128, 128], F32), ("ck", [256, 16, 128], F32), ("cv", [256, 16, 128], F32),
    ("ada_w", [2, D, 6 * D], F32), ("ada_b", [2, 6 * D], F32), ("norm1_g", [2, D], F32), ("norm2_g", [2, D], F32),
    ("final_g", [D], F32), ("dn_w_in", [1, D, 12416], F32), ("dn_conv_w", [1, 5, 8192], F32),
    ("dn_a_log", [1, 2, 32], F32), ("dn_dt_bias", [1, 2, 32], F32), ("dn_norm_g", [1, 128], F32),
    ("dn_w_o", [1, 4096, D], F32), ("na_w_qkv", [1, D, 3 * D], F32), ("na_w_o", [1, D, D], F32),
    ("peer_w_q", [2, D, 2048], F32), ("peer_keys", [2, 8, 2, 128, 128], F32),
    ("peer_u", [2, 16384, D], F32), ("peer_v", [2, 16384, D], F32),
    ("dncst", [128, 6, 128], F32), ("rpbx", [128, 16, 14, 64], F32), ("okm", [128, 64], F32),
]
_OUT_SPECS = [
    ("yp", [NPS * LP, D], F32), ("ys", [LS, D], F32), ("nsd", [NPS, 2, 32, 128, 128], F32),
    ("nck", [NPS * LP, D], F32), ("ncv", [NPS * LP, D], F32),
]


def build_nc(stop_after=99):
    nc = bass.Bass("TRN2", target_bir_lowering=False)
    cx = Ctx()
    cx.nc = nc
    for name, shape, dt in _IN_SPECS:
        setattr(cx, name, nc.dram_tensor(name, shape, dt, kind="ExternalInput").ap())
    for name, shape, dt in _OUT_SPECS:
        setattr(cx, name, nc.dram_tensor(name, shape, dt, kind="ExternalOutput").ap())
    cx.mod = dram(nc, "mod", [2, 2, 6 * D], F32)
    cx.projT = dram(nc, "projT", [64, 128, LTOT], F32)
    cx.z = dram(nc, "zs", [LTOT, 4096], BF16)
    cx.bg = dram(nc, "bg", [LTOT, 128], F32)
    cx.qT = dram(nc, "qT", [16, 128, LTOT], BF16)
    cx.kT = dram(nc, "kT", [16, 128, LTOT], BF16)
    cx.ktok = dram(nc, "ktok", [LTOT, 16, 128], BF16)
    cx.vtok = dram(nc, "vtok", [LTOT, 32, 128], BF16)
    cx.odn = dram(nc, "odn", [2, LTOT, 4096], F32)
    cx.xres = [dram(nc, "xres0", [LTOT, D], F32)]
    cx.xmid = dram(nc, "xmid", [LTOT, D], F32)
    cx.qnT = dram(nc, "qnT", [16, 128, LTOT], BF16)
    cx.knT = dram(nc, "knT", [16, 128, LTOT], BF16)
    cx.vn = dram(nc, "vn", [LTOT, D], BF16)
    cx.oT = dram(nc, "oT", [16, 128, LTOT], BF16)
    with contextlib.ExitStack() as es:
        em = Em(nc, es)
        em.init_psum()
        cx.identf = em.sb("identf", [128, 128], F32)
        em.dma(cx.identf[:], cx.dncst[:, 0, :])
        cx.identb = em.sb("identb", [128, 128], BF16)
        em.copy(cx.identb[:], cx.identf[:])
        cx.onesb = em.sb("onesb", [128, 128], BF16)
        em.memset(cx.onesb[:], 1.0)
        phase_adaln(em, cx)
        if stop_after >= 1:
            phase_dn_proj(em, cx)
            phase_dn_conv(em, cx)
            phase_dn_scan(em, cx)
        if stop_after >= 2:
            phase_post(em, cx, 0)
        if stop_after >= 3:
            phase_na(em, cx)
        if stop_after >= 4:
            phase_post(em, cx, 1)
        em.barrier()
        cx.n_inst = em.n_inst
    return nc, cx


def _rpb_layout(rpb):
    kc = np.arange(64)[:, None]
    qc = np.arange(64)[None, :]
    dc = np.clip(kc - qc, -15, 15) + 15
    out = np.empty((2, 64, 16, 14, 64), np.float32)
    for j in range(2):
        g = rpb[:, j:j + 14, :]
        out[j] = np.transpose(g[:, :, dc], (2, 0, 1, 3))
    return np.ascontiguousarray(out.reshape(128, 16, 14, 64))


def _ok_mask():
    qc = np.arange(64)
    c0 = np.clip(qc - 8, 0, 48)
    kc = np.arange(64)[:, None]
    ok = ((kc >= c0[None, :]) & (kc < c0[None, :] + 16)).astype(np.float32)
    return np.ascontiguousarray(np.concatenate([ok, ok], axis=0))


_NC_CACHE = {}


def kernel(x_prompt, x_sample, c, state_delta, cache_k, cache_v, c_ctx, ada_w, ada_b, norm1_g, norm2_g,
           final_g, dn_w_in, dn_conv_w, dn_a_log, dn_dt_bias, dn_norm_g, dn_w_o, na_w_qkv, na_rpb, na_w_o,
           peer_w_q, peer_keys, peer_u, peer_v):
    f = lambda a: np.ascontiguousarray(np.asarray(a, dtype=np.float32))
    x_prompt, x_sample, c, state_delta, cache_k, cache_v, c_ctx = map(f, (x_prompt, x_sample, c, state_delta, cache_k, cache_v, c_ctx))
    shared = dict(ada_w=f(ada_w), ada_b=f(ada_b), norm1_g=f(norm1_g), norm2_g=f(norm2_g), final_g=f(final_g),
                  dn_w_in=f(dn_w_in), dn_conv_w=f(dn_conv_w), dn_a_log=f(dn_a_log), dn_dt_bias=f(dn_dt_bias),
                  dn_norm_g=f(dn_norm_g), dn_w_o=f(dn_w_o), na_w_qkv=f(na_w_qkv), na_w_o=f(na_w_o),
                  peer_w_q=f(peer_w_q), peer_keys=f(peer_keys), peer_u=f(peer_u), peer_v=f(peer_v),
                  dncst=dn_host_consts(), rpbx=_rpb_layout(f(na_rpb)[0]), okm=_ok_mask())
    if "nc" not in _NC_CACHE:
        _NC_CACHE["nc"] = build_nc()
    nc, cx = _NC_CACHE["nc"]
    in_maps = []
    for core in range(8):
        b = core % 2
        m = dict(shared)
        m["xp"] = np.ascontiguousarray(x_prompt[core * NPS:(core + 1) * NPS].reshape(NPS * LP, D))
        m["xs"] = x_sample[b]
        m["cvec"] = np.ascontiguousarray(np.stack([c_ctx, c[b]], axis=0))
        m["s0"] = np.ascontiguousarray(state_delta[b, 0])
        m["ck"] = np.ascontiguousarray(cache_k[b, 0])
        m["cv"] = np.ascontiguousarray(cache_v[b, 0])
        in_maps.append(m)
    res = run_bass_kernel_spmd(nc, in_maps, core_ids=list(range(8)))
    r = res.results
    y_prompt = np.concatenate([r[i]["yp"].reshape(NPS, LP, D) for i in range(8)], axis=0)
    y_sample = np.stack([r[0]["ys"], r[1]["ys"]], axis=0)
    nsd = np.concatenate([r[i]["nsd"] for i in range(8)], axis=0)[:, None]
    nck = np.concatenate([r[i]["nck"].reshape(NPS, LP, 16, 128) for i in range(8)], axis=0)[:, None]
    ncv = np.concatenate([r[i]["ncv"].reshape(NPS, LP, 16, 128) for i in range(8)], axis=0)[:, None]
    return (y_prompt.astype(np.float32), y_sample.astype(np.float32), nsd.astype(np.float32),
            nck.astype(np.float32), ncv.astype(np.float32))
```

```python
from concourse.bass_utils import run_bass_kernel_spmd
import contextlib
import numpy as np
import concourse.bass as bass
import concourse.mybir as mybir

F32 = mybir.dt.float32
BF16 = mybir.dt.bfloat16
I32 = mybir.dt.int32
AF = mybir.ActivationFunctionType
ALU = mybir.AluOpType
AX = mybir.AxisListType


class Em:
    ENG = ("pe", "dve", "act", "pool", "sp")

    def __init__(self, nc, es):
        self.nc = nc
        self.es = es
        self.eng = {"pe": nc.tensor, "dve": nc.vector, "act": nc.scalar, "pool": nc.gpsimd, "sp": nc.sync}
        self.sem = {e: es.enter_context(nc.semaphore("s_" + e)) for e in self.ENG if e != "sp"}
        self.cnt = {e: 0 for e in self.ENG}
        self.seen = {e: {} for e in self.ENG}
        self.last_w = {}
        self.readers = {}
        self.dsem = {}
        self.dfree = []
        self.dscopes = [set()]
        self.n_inst = 0
        self.psum = []
        self.ps_i = 0
        self.uid = 0

    def sb(self, name, shape, dt=F32, es=None):
        self.uid += 1
        return (es or self.es).enter_context(self.nc.sbuf_tensor(f"{name}_{self.uid}", list(shape), dt))

    def init_psum(self):
        for i in range(8):
            self.psum.append(self.es.enter_context(self.nc.psum_tensor(f"ps{i}", [128, 512], F32)))

    def ps(self):
        t = self.psum[self.ps_i % 8]
        self.ps_i += 1
        return t

    @staticmethod
    def key(ap):
        if isinstance(ap, str):
            return ap
        return ap.tensor.name

    def _deps(self, e, reads, writes):
        need = {}
        def add(tok):
            k, v = tok
            if k == "pe" and e == "pe":
                return
            if need.get(k, 0) < v:
                need[k] = v
        for r in reads:
            t = self.last_w.get(r)
            if t:
                add(t)
            if r.startswith("ps"):
                for t in self.readers.get(r, ()):
                    if t[0] != e:
                        add(t)
        for w in writes:
            t = self.last_w.get(w)
            if t:
                add(t)
            for t in self.readers.get(w, ()):
                add(t)
        for k, v in need.items():
            if k.startswith("dma:"):
                v = self.dsem[k[4:]][1]
            if self.seen[e].get(k, 0) < v:
                sem = self.dsem[k[4:]][0] if k.startswith("dma:") else self.sem[k]
                self.eng[e].wait_ge(sem, v)
                self.seen[e][k] = v
                self.n_inst += 1

    def _commit(self, tok, reads, writes):
        for w in writes:
            self.last_w[w] = tok
            self.readers[w] = set()
        for r in reads:
            if r not in writes:
                self.readers.setdefault(r, set()).add(tok)

    def _rw(self, kw, extra_r=(), extra_w=()):
        reads, writes = [], []
        for k, v in kw.items():
            if isinstance(v, bass.AP):
                if k in ("out", "accum_out"):
                    writes.append(self.key(v))
                else:
                    reads.append(self.key(v))
        reads += [self.key(x) for x in extra_r]
        writes += [self.key(x) for x in extra_w]
        return reads, writes

    def op(self, e, name, extra_r=(), extra_w=(), **kw):
        reads, writes = self._rw(kw, extra_r, extra_w)
        self._deps(e, reads, writes)
        inst = getattr(self.eng[e], name)(**kw)
        self.cnt[e] += 1
        inst.then_inc(self.sem[e], 1)
        self.n_inst += 1
        self._commit((e, self.cnt[e]), reads, writes)
        return inst

    def mm(self, out, lhsT, rhs, start=True, stop=True, transpose=False, **kw):
        reads = [self.key(lhsT), self.key(rhs)]
        writes = [self.key(out)]
        self._deps("pe", reads, writes)
        if transpose:
            inst = self.nc.tensor.transpose(out, lhsT, rhs, **kw)
        else:
            inst = self.nc.tensor.matmul(out, lhsT=lhsT, rhs=rhs, start=start, stop=stop, **kw)
        self.n_inst += 1
        if stop:
            self.cnt["pe"] += 1
            inst.then_inc(self.sem["pe"], 1)
            tok = ("pe", self.cnt["pe"])
        else:
            tok = ("pe", self.cnt["pe"] + 1)
        self._commit(tok, reads, writes)
        return inst

    def dma(self, out, in_, stream=None, q="sp", **kw):
        reads = [self.key(in_)]
        writes = [self.key(out)]
        if stream is None:
            stream = self.key(out) if "DRam" not in type(out.tensor).__name__ else self.key(in_)
        if stream not in self.dsem:
            self._new_stream(stream)
        self._deps(q, reads, writes)
        inst = self.eng[q].dma_start(out=out, in_=in_, **kw)
        self.dsem[stream][1] += 16
        inst.then_inc(self.dsem[stream][0], 16)
        self.n_inst += 1
        self._commit(("dma:" + stream, self.dsem[stream][1]), reads, writes)
        return inst

    def _new_stream(self, stream):
        if self.dfree:
            self.dsem[stream] = self.dfree.pop()
        else:
            self.dsem[stream] = [self.es.enter_context(self.nc.semaphore("d%d" % len(self.dsem))), 0]
        self.dscopes[-1].add(stream)

    def barrier(self):
        for e in self.ENG:
            for p in self.sem:
                v = self.cnt[p]
                if v and self.seen[e].get(p, 0) < v:
                    self.eng[e].wait_ge(self.sem[p], v)
                    self.seen[e][p] = v
            for s, (sem, v) in self.dsem.items():
                k = "dma:" + s
                if v and self.seen[e].get(k, 0) < v:
                    self.eng[e].wait_ge(sem, v)
                    self.seen[e][k] = v
        self.last_w.clear()
        self.readers.clear()

    def act(self, out, in_, func, e="act", **kw):
        return self.op(e, "activation", out=out, in_=in_, func=func, **kw)

    def tt(self, out, in0, in1, op, e="dve"):
        return self.op(e, "tensor_tensor", out=out, in0=in0, in1=in1, op=op)

    def ts(self, out, in0, s1, op0, s2=None, op1=None, e="dve", **kw):
        if op1 is None:
            return self.op(e, "tensor_scalar", out=out, in0=in0, scalar1=s1, scalar2=None, op0=op0, **kw)
        return self.op(e, "tensor_scalar", out=out, in0=in0, scalar1=s1, scalar2=s2, op0=op0, op1=op1, **kw)

    def stt(self, out, in0, scalar, in1, op0, op1, e="dve"):
        return self.op(e, "scalar_tensor_tensor", out=out, in0=in0, scalar=scalar, in1=in1, op0=op0, op1=op1)

    def copy(self, out, in_, e="dve"):
        if e == "act":
            return self.op("act", "activation", out=out, in_=in_, func=AF.Copy)
        return self.op(e, "tensor_copy", out=out, in_=in_)

    def memset(self, ap, val, e="dve"):
        reads, writes = [], [self.key(ap)]
        self._deps(e, reads, writes)
        inst = self.eng[e].memset(ap, val)
        self.cnt[e] += 1
        inst.then_inc(self.sem[e], 1)
        self.n_inst += 1
        self._commit((e, self.cnt[e]), reads, writes)
        return inst


def _dbg(self, name, ap, dt=None):
    shape = list(ap.shape)
    d = self.nc.dram_tensor("dbg_" + name, shape, dt or ap.dtype, kind="ExternalOutput").ap()
    self.dma(d, ap)
Em.dbg = _dbg


@contextlib.contextmanager
def _scope(self):
    with contextlib.ExitStack() as es:
        self.dscopes.append(set())
        yield es
        self.barrier()
        for stream in self.dscopes.pop():
            ent = self.dsem.pop(stream)
            self.dfree.append(ent)
            for e in self.ENG:
                self.seen[e].pop("dma:" + stream, None)
Em.scope = _scope


def _rot(self, es, name, shape, dt=F32, n=2):
    pools = getattr(es, "_pools", None)
    if pools is None:
        pools = {}
        es._pools = pools
    ent = pools.get(name)
    if ent is None:
        ent = [[self.sb(name, shape, dt, es) for _ in range(n)], 0]
        pools[name] = ent
    t = ent[0][ent[1] % len(ent[0])]
    ent[1] += 1
    return t
Em.rot = _rot


def _allgather(self, out, in_, groups, stream="cc"):
    reads = [self.key(in_)]
    writes = [self.key(out)]
    if stream not in self.dsem:
        self._new_stream(stream)
    self._deps("pool", reads, writes)
    inst = self.nc.gpsimd.collective_compute("AllGather", op=ALU.bypass, replica_groups=groups, ins=[in_], outs=[out])
    self.dsem[stream][1] += 16
    inst.then_inc(self.dsem[stream][0], 16)
    self.n_inst += 1
    self._commit(("dma:" + stream, self.dsem[stream][1]), reads, writes)
    return inst
Em.allgather = _allgather


def _load_w(self, es, dst, src, kc, ncols, piece=256):
    if not isinstance(dst, bass.AP):
        dst = dst[:]
    self.dma(dst, src.rearrange("(c p) n -> p c n", p=128), q="pool")
Em.load_w = _load_w


def peer_keysT(em, es, keys_l, ident_f):
    kf = em.sb("keys_f", [128, 16, 128], F32, es)
    em.dma(kf[:], keys_l.rearrange("h p n d -> n (h p) d"))
    keysT = em.sb("keysT", [128, 16, 128], BF16, es)
    for g in range(4):
        pt = em.ps()
        for k in range(4):
            c = g * 4 + k
            em.mm(pt[:, k * 128:(k + 1) * 128], kf[:, c, :], ident_f[:], transpose=True)
        em.copy(keysT[:, g * 4:(g + 1) * 4, :], pt[:].rearrange("p (c n) -> p c n", c=4), e="act")
    return keysT


def peer_pass(em, nc, es0, DC, hT2, ntp, wq_l, keysT, u_l, v_l, ident_b, acc_out, NI=128, IB=2, dbg=False):
    D = DC * 128
    NT = ntp * 128
    with em.scope() as es:
        stok = [em.sb("stok", [128, 8, 2, 128], F32, es) for _ in range(ntp)]
        diag = [em.sb("diag", [128, 8, 128], BF16, es) for _ in range(ntp)]
        _cm = em.scope()
        es_q = _cm.__enter__()
        qT = em.sb("qT", [128, 16, NT], BF16, es_q)
        wqb = [em.sb("wqb", [128, DC, 256], BF16, es_q) for _ in range(2)]
        for c in range(16):
            wb = wqb[(c // 2) % 2]
            if c % 2 == 0:
                em.load_w(es_q, wb, wq_l[:, c * 128:(c + 2) * 128], DC, 256)
            pq = em.ps()
            for dc in range(DC):
                em.mm(pq[:, 0:NT], wb[:, dc, (c % 2) * 128:(c % 2 + 1) * 128], hT2[:, dc, :], start=(dc == 0), stop=(dc == DC - 1))
            em.copy(qT[:, c, :], pq[:, 0:NT], e="act")
        with em.scope() as es2:
            top = em.sb("top", [128, 2, 16], F32, es2)
            work = em.sb("work", [128, 128], F32, es2)
            cand = em.sb("cand", [128, 16, 16], F32, es2)
            cand2 = em.sb("cand2", [128, 256], F32, es2)
            c24 = em.sb("c24", [128, 24], F32, es2)
            tau = em.sb("tau", [128, 8], F32, es2)
            zs = em.sb("zs", [128, 8], F32, es2)
            ejunk = em.sb("ejunk", [128, 16], F32, es2)
            ntau = em.sb("ntau", [128, 1], F32, es2)
            for tt in range(ntp):
                st = stok[tt]
                for g in range(4):
                    pt = em.ps()
                    for k in range(4):
                        c = g * 4 + k
                        em.mm(pt[:, k * 128:(k + 1) * 128], qT[:, c, tt * 128:(tt + 1) * 128], keysT[:, c, :])
                    em.copy(st[:, 2 * g:2 * g + 2, :, :], pt[:].rearrange("p (h q n) -> p h q n", h=2, q=2), e="act")
                for h in range(8):
                    for p in range(2):
                        src = st[:, h, p, :]
                        em.op("dve", "max", out=top[:, p, 0:8], in_=src)
                        em.op("dve", "match_replace", out=work[:], in_to_replace=top[:, p, 0:8], in_values=src, imm_value=-1e30)
                        em.op("dve", "max", out=top[:, p, 8:16], in_=work[:])
                    em.tt(cand[:], top[:, 0, :].unsqueeze(2).to_broadcast([128, 16, 16]),
                          top[:, 1, :].unsqueeze(1).to_broadcast([128, 16, 16]), ALU.add)
                    cf = cand[:].rearrange("p a b -> p (a b)")
                    em.op("dve", "max", out=c24[:, 0:8], in_=cf)
                    em.op("dve", "match_replace", out=cand2[:], in_to_replace=c24[:, 0:8], in_values=cf, imm_value=-1e30)
                    em.op("dve", "max", out=c24[:, 8:16], in_=cand2[:])
                    em.op("dve", "match_replace", out=cand2[:], in_to_replace=c24[:, 8:16], in_values=cand2[:], imm_value=-1e30)
                    em.op("dve", "max", out=c24[:, 16:24], in_=cand2[:])
                    em.ts(tau[:, h:h + 1], c24[:, 15:16], c24[:, 16:17], ALU.add, 0.5, ALU.mult)
                    em.ts(ntau[:], tau[:, h:h + 1], -1.0, ALU.mult)
                    em.act(ejunk[:], c24[:, 0:16], AF.Exp, bias=ntau[:, 0:1], accum_out=zs[:, h:h + 1])
                em.tt(st[:, :, 0, :], st[:, :, 0, :], tau[:].unsqueeze(2).to_broadcast([128, 8, 128]), ALU.subtract)
                em.op("dve", "reciprocal", out=zs[:], in_=zs[:])
                for h in range(8):
                    em.ts(diag[tt][:, h, :], ident_b[:], zs[:, h:h + 1], ALU.mult)
                if dbg and tt == 0:
                    em.dbg("tau", tau[:]); em.dbg("zs", zs[:]); em.dbg("stok", st[:]); em.dbg("c24", c24[:]); em.dbg("top", top[:])
                    em.dbg("qT", qT[:])
        _cm.__exit__(None, None, None)
        urow = [em.sb("urow", [128, D], BF16, es) for _ in range(3)]
        uT = [em.sb("uT", [128, DC, 128], BF16, es) for _ in range(2)]
        vblk = [[em.sb("vblk", [128, D], BF16, es) for _ in range(IB)] for _ in range(2)]
        gS = [em.sb("gS", [128, NT], BF16, es) for _ in range(2)]
        AT = [[em.sb("AT", [128, NT], BF16, es) for _ in range(IB)] for _ in range(2)]
        Pp = [em.sb("Pp", [128, 8, 128], F32, es) for _ in range(2)]
        Ee = [em.sb("Ee", [128, 8, 128], BF16, es) for _ in range(2)]
        Gg = [em.sb("Gg", [128, 8, 128], BF16, es) for _ in range(2)]
        Mk = [em.sb("Mk", [128, 8, 128], BF16, es) for _ in range(2)]
        for a in acc_out:
            em.memset(a[:], 0.0, e="pool")
        nblk = NI // IB
        k2 = 0

        def load(i):
            b, ii = divmod(i, IB)
            em.dma(urow[i % 3][:], u_l[i * 128:(i + 1) * 128, :], q="pool")
            em.dma(vblk[b % 2][ii][:], v_l[i * 128:(i + 1) * 128, :], q="pool")

        load(0)
        load(1)
        for b in range(nblk):
            vb = vblk[b % 2]
            at = AT[b % 2]
            for ii in range(IB):
                i = b * IB + ii
                ur = urow[i % 3]
                ut = uT[i % 2]
                for g0 in range(0, DC, 8):
                    n = min(8, DC - g0)
                    pt = em.ps()
                    ptb = pt[:].bitcast(BF16)
                    for k in range(n):
                        em.mm(ptb[:, k * 128:(k + 1) * 128], ur[:, (g0 + k) * 128:(g0 + k + 1) * 128], ident_b[:], transpose=True)
                    em.copy(ut[:, g0:g0 + n, :], ptb[:, 0:n * 128].rearrange("p (c e) -> p c e", c=n), e="act")
                if i + 2 < NI:
                    load(i + 2)
                pS = em.ps()
                for dc in range(DC):
                    em.mm(pS[:, 0:NT], ut[:, dc, :], hT2[:, dc, :], start=(dc == 0), stop=(dc == DC - 1))
                gs = gS[i % 2]
                em.act(gs[:], pS[:, 0:NT], AF.Gelu)
                pG = em.ps()
                for tt in range(ntp):
                    st = stok[tt]
                    pp, ee, gg = Pp[k2 % 2], Ee[k2 % 2], Gg[k2 % 2]
                    k2 += 1
                    em.tt(pp[:], st[:, :, 1, :], st[:, :, 0, i:i + 1].to_broadcast([128, 8, 128]), ALU.add, e="pool")
                    em.act(ee[:], pp[:], AF.Exp)
                    mk = Mk[k2 % 2]
                    em.ts(mk[:], pp[:], 0.0, ALU.is_ge)
                    em.tt(gg[:], mk[:], ee[:], ALU.mult)
                    for h in range(8):
                        em.mm(pG[:, tt * 128:(tt + 1) * 128], gg[:, h, :], diag[tt][:, h, :], start=(h == 0), stop=(h == 7))
                em.tt(at[ii][:], pG[:, 0:NT], gs[:], ALU.mult)
                if dbg and i == 0:
                    em.dbg("gs", gs[:]); em.dbg("at", at[ii][:]); em.dbg("pp", Pp[0][:]); em.dbg("ee", Ee[0][:]); em.dbg("gg", Gg[0][:])
            for tt in range(ntp):
                banks = [em.ps() for _ in range((D + 511) // 512)]
                for cc, bk in enumerate(banks):
                    w = min(512, D - cc * 512)
                    for ii in range(IB):
                        em.mm(bk[:, 0:w], at[ii][:, tt * 128:(tt + 1) * 128], vb[ii][:, cc * 512:cc * 512 + w],
                              start=(ii == 0), stop=(ii == IB - 1))
                for cc, bk in enumerate(banks):
                    w = min(512, D - cc * 512)
                    em.tt(acc_out[tt][:, cc * 512:cc * 512 + w], acc_out[tt][:, cc * 512:cc * 512 + w], bk[:, 0:w], ALU.add,
                          e="dve")
                if dbg and b == 0 and tt == 0:
                    em.dbg("acc0", acc_out[0][:]); em.dbg("vb0", vb[0][:]); em.dbg("vb3", vb[3][:])
                    tmpd = em.sb("tmpd", [128, 512], F32, es); em.copy(tmpd[:], banks[0][:]); em.dbg("bank", tmpd[:])

NEG = -30000.0


def dn_host_consts():
    p = np.arange(128)[:, None]
    f = np.arange(128)[None, :]
    c = np.zeros((128, 6, 128), np.float32)
    c[:, 0] = (p == f)
    c[:, 1] = (p <= f)
    c[:, 2] = (p >= f)
    c[:, 3] = np.where(p > f, 0.0, NEG)
    c[:, 4] = np.where(p < f, 0.0, NEG)
    c[:, 5] = 1.0
    return c


class DnState:
    pass


def dn_setup(em, es, consts_d, NH, aug):
    st = DnState()
    st.NH = NH
    st.W = 256 if aug else 128
    st.cst = em.sb("dncst", [128, 6, 128], F32, es)
    em.dma(st.cst[:], consts_d)
    st.identb = em.sb("dnidb", [128, 128], BF16, es)
    em.copy(st.identb[:], st.cst[:, 0, :])
    st.negT = em.sb("dnnegT", [128, 2, 128], F32, es)
    em.ts(st.negT[:], st.cst[:, 1:3, :], -1.0, ALU.add, -NEG, ALU.mult)
    st.S = [[em.sb("S", [128, st.W], F32, es) for _ in range(NH)] for _ in range(2)]
    st.Sb = [[em.sb("Sb", [128, st.W], BF16, es) for _ in range(NH)] for _ in range(2)]
    return st


def dn_chunk(em, es, st, d, kT, qT, ktok, vtok, beta, g, rep, want_o, o_out, aug=False, dbg=None, stage=99):
    NH = st.NH
    W = st.W
    ident = st.cst[:, 0, :]
    ones = st.cst[:, 5, :]
    Mincl = st.cst[:, 1 + d, :]
    negS = st.cst[:, 3 + d, :]
    negT = st.negT[:, d, :]
    pc = em.ps()
    em.mm(pc[:, 0:NH], Mincl, g[:, :])
    em.mm(pc[:, 128:128 + NH], ones, g[:, :])
    gc = em.rot(es, "gc", [128, 6, NH], F32, 2)
    em.copy(gc[:, 0, :], pc[:, 0:NH])
    em.ts(gc[:, 1, :], pc[:, 0:NH], -1.0, ALU.mult)
    em.act(gc[:, 2, :], pc[:, 0:NH], AF.Exp)
    em.tt(gc[:, 2, :], gc[:, 2, :], beta[:, :], ALU.mult)
    em.tt(gc[:, 5, :], pc[:, 128:128 + NH], gc[:, 0, :], ALU.subtract)
    em.act(gc[:, 3, :], gc[:, 5, :], AF.Exp)
    em.act(gc[:, 4, :], pc[:, 128:128 + NH], AF.Exp)
    if stage < 1:
        return
    nhk = NH // rep
    for hk in range(nhk):
        pk = em.ps()
        em.mm(pk[:, 0:128], kT[:, hk, :], kT[:, hk, :])
        em.mm(pk[:, 128:256], kT[:, hk, :], qT[:, hk, :])
        kkq = em.rot(es, "kkq", [128, 256], F32, 2)
        em.copy(kkq[:], pk[:, 0:256], e="act")
        for r in range(rep):
            h = hk * rep + r
            S, Sb = st.S[d][h], st.Sb[d][h]
            gM = em.rot(es, "gM", [128, 128], F32, 2)
            em.ts(gM[:], Mincl, g[:, h:h + 1], ALU.mult)
            pg = em.ps()
            em.mm(pg[:, 0:128], ones, gM[:])
            t1 = em.rot(es, "t1", [128, 128], F32, 2)
            em.stt(t1[:], pg[:, 0:128], -1.0, negS, ALU.mult, ALU.add)
            em.act(t1[:], t1[:], AF.Exp, bias=gc[:, 0, h:h + 1])
            A = em.rot(es, "A", [128, 128], F32, 2)
            em.stt(A[:], t1[:], beta[:, h:h + 1], kkq[:, 0:128], ALU.mult, ALU.mult)
            t2 = em.rot(es, "t2", [128, 128], F32, 2)
            em.tt(t2[:], pg[:, 0:128], negT, ALU.add)
            em.act(t2[:], t2[:], AF.Exp, bias=gc[:, 1, h:h + 1])
            if want_o:
                qkT = em.rot(es, "qkT", [128, 128], BF16, 2)
                em.tt(qkT[:], t2[:], kkq[:, 128:256], ALU.mult)
                eg = em.rot(es, "eg", [128, 128], F32, 2)
                em.act(eg[:], pg[:, 0:128], AF.Exp)
                qgT = em.rot(es, "qgT", [128, 128], BF16, 2)
                em.tt(qgT[:], qT[:, hk, :], eg[:], ALU.mult)
            if stage < 2:
                continue
            pa = em.ps()
            em.mm(pa[:, 0:128], A[:], ident, transpose=True)
            B = em.rot(es, "B", [128, 128], F32, 3)
            BT = em.rot(es, "BT", [128, 128], F32, 3)
            P = em.rot(es, "P", [128, 128], F32, 3)
            em.ts(B[:], A[:], -1.0, ALU.mult)
            em.ts(BT[:], pa[:, 0:128], -1.0, ALU.mult)
            em.tt(P[:], BT[:], ident, ALU.add)
            if stage < 2.2:
                continue
            for lvl in range(1, 7 if stage >= 2.6 else 2):
                last = lvl == 6
                if stage < 2.4 and lvl >= 1:
                    pb = em.ps()
                    em.mm(pb[:, 0:128], BT[:], B[:])
                    continue
                pb = em.ps()
                em.mm(pb[:, 0:128], BT[:], B[:])
                if not last:
                    em.mm(pb[:, 128:256], B[:], BT[:])
                if stage < 2.42:
                    continue
                B2 = em.rot(es, "B", [128, 128], F32, 3)
                em.copy(B2[:], pb[:, 0:128], e="act")
                if stage < 2.44:
                    B = B2
                    continue
                if not last:
                    BT2 = em.rot(es, "BT", [128, 128], F32, 3)
                    em.ts(BT2[:], pb[:, 128:256], 1.0, ALU.mult) if "dve" == "dve" else em.copy(BT2[:], pb[:, 128:256], e="act")
                    BT = BT2
                B = B2
                if stage < 2.5:
                    continue
                pp = em.ps()
                em.mm(pp[:, 0:128], B[:], P[:])
                P2 = em.rot(es, "P", [128, 128], F32, 3)
                em.tt(P2[:], P[:], pp[:, 0:128], ALU.add)
                P = P2
            if stage < 3:
                continue
            Pb = em.rot(es, "Pb", [128, 128], BF16, 2)
            em.copy(Pb[:], P[:], e="act")
            bv = em.rot(es, "bv", [128, 128], BF16, 2)
            em.ts(bv[:], vtok[:, h, :], beta[:, h:h + 1], ALU.mult)
            bk = em.rot(es, "bk", [128, 128], BF16, 2)
            em.ts(bk[:], ktok[:, hk, :], gc[:, 2, h:h + 1], ALU.mult)
            kd = em.rot(es, "kd", [128, 128], BF16, 2)
            em.ts(kd[:], ktok[:, hk, :], gc[:, 3, h:h + 1], ALU.mult)
            pw = em.ps()
            em.mm(pw[:, 0:128], bk[:], Pb[:])
            nwkT = em.rot(es, "nwkT", [128, 128], BF16, 2)
            em.ts(nwkT[:], pw[:, 0:128], -1.0, ALU.mult)
            pwv = em.ps()
            em.mm(pwv[:, 0:128], Pb[:], bv[:], start=True, stop=False)
            em.mm(pwv[:, 0:128], nwkT[:], Sb[:, 0:128], start=False, stop=True)
            if aug:
                em.mm(pwv[:, 128:256], nwkT[:], Sb[:, 128:256], start=True, stop=True)
            wb = em.rot(es, "wb", [128, W], BF16, 2)
            em.copy(wb[:], pwv[:, 0:W], e="act")
            if want_o:
                po = em.ps()
                em.mm(po[:, 0:128], qgT[:], Sb[:, 0:128], start=True, stop=False)
                em.mm(po[:, 0:128], qkT[:], wb[:, 0:128], start=False, stop=True)
                em.copy(o_out[:, h, :], po[:, 0:128], e="act")
            pS = em.ps()
            em.mm(pS[:, 0:W], kd[:], wb[:])
            em.stt(S[:], S[:], gc[:, 4, h:h + 1], pS[:, 0:W], ALU.mult, ALU.add)
            em.copy(Sb[:], S[:], e="act")
            if dbg is not None and h == 0:
                dbg(dict(A=A, P=P, t2=t2, wb=wb, kkq=kkq, gc=gc))


D = 2048
DC = 16
LP = 256
NPS = 2
LS = 4096
LTOT = NPS * LP + LS
DN_QKV = 8192
EPS = 1e-6


def dram(nc, name, shape, dt):
    return nc.dram_tensor(name, list(shape), dt, kind="Internal").ap()


class Ctx:
    pass


def bcast_row(ap_row, n=128):
    a = ap_row.partition_broadcast(n)
    if len(a.shape) == 3:
        a = a.rearrange("p o d -> p (o d)")
    return a


def load_mod(em, es, cx, layer, variant, k, name):
    t = em.rot(es, name, [128, D], F32, 1)
    em.dma(t[:], bcast_row(cx.mod[layer, variant:variant + 1, k * D:(k + 1) * D]))
    return t


def make_gm(em, es, cx, layer, variant, which, name):
    sh = load_mod(em, es, cx, layer, variant, 3 * which + 0, name + "sh")
    sc = load_mod(em, es, cx, layer, variant, 3 * which + 1, name + "sc")
    g = em.rot(es, name + "g", [128, D], F32, 1)
    gsrc = (cx.norm1_g if which == 0 else cx.norm2_g)[layer:layer + 1, :]
    em.dma(g[:], bcast_row(gsrc))
    em.stt(sc[:], sc[:], 1.0, g[:], ALU.add, ALU.mult)
    return sc, sh


def norm_mod_T(em, es, cx, xt, gm, sh, hT, col0):
    junk = em.rot(es, "nm_junk", [128, D], BF16, 1)
    ss = em.rot(es, "nm_ss", [128, 1], F32, 2)
    em.act(junk[:], xt[:], AF.Square, accum_out=ss[:])
    rs = em.rot(es, "nm_rs", [128, 1], F32, 2)
    em.ts(rs[:], ss[:], 1.0 / D, ALU.mult, EPS, ALU.add)
    em.act(rs[:], rs[:], AF.Sqrt)
    em.op("dve", "reciprocal", out=rs[:], in_=rs[:])
    t = em.rot(es, "nm_t", [128, D], F32, 1)
    em.stt(t[:], xt[:], rs[:, 0:1], gm[:], ALU.mult, ALU.mult)
    hb = em.rot(es, "nm_hb", [128, D], BF16, 2)
    em.tt(hb[:], t[:], sh[:], ALU.add, e="pool")
    for g0 in range(0, DC, 8):
        pt = em.ps()
        ptb = pt[:].bitcast(BF16)
        for k in range(8):
            em.mm(ptb[:, k * 128:(k + 1) * 128], hb[:, (g0 + k) * 128:(g0 + k + 1) * 128], cx.identb[:], transpose=True)
        em.copy(hT[:, g0:g0 + 8, col0:col0 + 128], ptb[:].rearrange("p (c t) -> p c t", c=8), e="act")


def x_src(cx, layer_in, tok0):
    if layer_in == 0:
        if tok0 < NPS * LP:
            return cx.xp[tok0:tok0 + 128, :]
        return cx.xs[tok0 - NPS * LP:tok0 - NPS * LP + 128, :]
    return cx.xres[layer_in - 1][tok0:tok0 + 128, :]


def phase_adaln(em, cx):
    nc = cx.nc
    with em.scope() as es:
        cv = em.sb("cv", [128, DC, 2], F32, es)
        with cx.nc.allow_non_contiguous_dma("tiny transposed load of conditioning vectors"):
            for v in range(2):
                em.dma(cv[:, :, v], cx.cvec[v, :].rearrange("(c p) -> p c", p=128))
        em.act(cv[:], cv[:], AF.Silu)
        for l in range(2):
            for cc in range(24):
                w = em.rot(es, "adaw", [128, DC, 512], F32, 2)
                em.dma(w[:], cx.ada_w[l, :, cc * 512:(cc + 1) * 512].rearrange("(c p) n -> p c n", p=128))
                b = em.rot(es, "adab", [2, 512], F32, 2)
                em.dma(b[:], bcast_row(cx.ada_b[l:l + 1, cc * 512:(cc + 1) * 512], 2))
                pm = em.ps()
                for dc in range(DC):
                    em.mm(pm[0:2, :], cv[:, dc, :], w[:, dc, :], start=(dc == 0), stop=(dc == DC - 1))
                m = em.rot(es, "adam", [2, 512], F32, 2)
                em.tt(m[:], pm[0:2, :], b[:], ALU.add)
                em.dma(cx.mod[l, :, cc * 512:(cc + 1) * 512], m[:])


def phase_dn_proj(em, cx):
    groups = [(0, NPS * LP, 0)] + [(NPS * LP + i * 512, 512, 1) for i in range(LS // 512)]
    with em.scope() as es:
        ea = em.sb("ea", [128, 64], F32, es)
        dtb = em.sb("dtb", [128, 64], F32, es)
        em.dma(ea[:], bcast_row(cx.dn_a_log.rearrange("o d h -> o (d h)")))
        em.dma(dtb[:], bcast_row(cx.dn_dt_bias.rearrange("o d h -> o (d h)")))
        em.act(ea[:], ea[:], AF.Exp)
        hT = em.sb("hT", [128, DC, 512], BF16, es)
        cur_var = None
        for (t0, n, var) in groups:
            if var != cur_var:
                gm, sh = make_gm(em, es, cx, 0, var, 0, "n1")
                cur_var = var
            for tt in range(n // 128):
                xt = em.rot(es, "xt", [128, D], F32, 2)
                em.dma(xt[:], x_src(cx, 0, t0 + tt * 128))
                norm_mod_T(em, es, cx, xt, gm, sh, hT, tt * 128)
            for c in range(64):
                if c % 2 == 0:
                    wb = em.rot(es, "winb", [128, DC, 256], BF16, 2)
                    em.load_w(es, wb, cx.dn_w_in[0, :, c * 128:(c + 2) * 128], DC, 256)
                pp = em.ps()
                for dc in range(DC):
                    em.mm(pp[:, 0:n], wb[:, dc, (c % 2) * 128:(c % 2 + 1) * 128], hT[:, dc, 0:n], start=(dc == 0), stop=(dc == DC - 1))
                pj = em.rot(es, "pj", [128, 512], F32, 3)
                em.copy(pj[:, 0:n], pp[:, 0:n], e="act")
                em.dma(cx.projT[c, :, t0:t0 + n], pj[:, 0:n])
            for cc in range(8):
                wz = em.rot(es, "wz", [128, DC, 512], BF16, 2)
                em.load_w(es, wz, cx.dn_w_in[0, :, DN_QKV + cc * 512:DN_QKV + (cc + 1) * 512], DC, 512)
                for tt in range(n // 128):
                    pz = em.ps()
                    for dc in range(DC):
                        em.mm(pz[:], hT[:, dc, tt * 128:(tt + 1) * 128], wz[:, dc, :], start=(dc == 0), stop=(dc == DC - 1))
                    zt = em.rot(es, "zt", [128, 512], BF16, 3)
                    em.act(zt[:], pz[:], AF.Silu)
                    em.dma(cx.z[t0 + tt * 128:t0 + (tt + 1) * 128, cc * 512:(cc + 1) * 512], zt[:])
            wba = em.rot(es, "wba", [128, DC, 128], BF16, 1)
            em.load_w(es, wba, cx.dn_w_in[0, :, DN_QKV + 4096:DN_QKV + 4096 + 128], DC, 128)
            for tt in range(n // 128):
                pb = em.ps()
                for dc in range(DC):
                    em.mm(pb[:, 0:128], hT[:, dc, tt * 128:(tt + 1) * 128], wba[:, dc, :], start=(dc == 0), stop=(dc == DC - 1))
                bg = em.rot(es, "bgt", [128, 128], F32, 2)
                em.act(bg[:, 0:64], pb[:, 0:64], AF.Sigmoid)
                tmp = em.rot(es, "bgtmp", [128, 64], F32, 2)
                em.tt(tmp[:], pb[:, 64:128], dtb[:], ALU.add)
                em.act(tmp[:], tmp[:], AF.Exp)
                em.act(tmp[:], tmp[:], AF.Ln, bias=1.0)
                em.stt(bg[:, 64:128], tmp[:], -1.0, ea[:], ALU.mult, ALU.mult)
                em.dma(cx.bg[t0 + tt * 128:t0 + (tt + 1) * 128, :], bg[:])


def phase_dn_conv(em, cx):
    seqs = [(i * LP, LP) for i in range(NPS)] + [(NPS * LP, LS)]
    with em.scope() as es:
        cw = em.sb("convw", [128, 5, 64], F32, es)
        with cx.nc.allow_non_contiguous_dma("small transposed conv weight load"):
            for k in range(5):
                em.dma(cw[:, k, :], cx.dn_conv_w[0, k, :].rearrange("(c p) -> p c", p=128))
        onesb = em.sb("onesb", [128, 128], BF16, es)
        em.memset(onesb[:], 1.0)
        for (t0, L) in seqs:
            for c in range(64):
                xin = em.rot(es, "cv_in", [128, LS + 4], F32, 2)
                em.memset(xin[:, 0:2], 0.0, e="pool")
                em.memset(xin[:, L + 2:L + 4], 0.0, e="pool")
                em.dma(xin[:, 2:L + 2], cx.projT[c, :, t0:t0 + L])
                acc = em.rot(es, "cv_acc", [128, LS], F32, 2)
                em.ts(acc[:, 0:L], xin[:, 0:L], cw[:, 0, c:c + 1], ALU.mult)
                for k in range(1, 5):
                    em.stt(acc[:, 0:L], xin[:, k:k + L], cw[:, k, c:c + 1], acc[:, 0:L], ALU.mult, ALU.add,
                           e=("dve" if k % 2 else "dve"))
                so = em.rot(es, "cv_so", [128, LS], BF16, 2)
                em.act(so[:, 0:L], acc[:, 0:L], AF.Silu)
                if c < 32:
                    sq = em.rot(es, "cv_sq", [128, LS], BF16, 1)
                    em.tt(sq[:, 0:L], so[:, 0:L], so[:, 0:L], ALU.mult, e="pool")
                    nrm = em.rot(es, "cv_nrm", [128, LS], BF16, 2)
                    for b0 in range(0, L, 512):
                        w = min(512, L - b0)
                        pn = em.ps()
                        em.mm(pn[:, 0:w], onesb[:], sq[:, b0:b0 + w])
                        rn = em.rot(es, "cv_rn", [128, 512], F32, 2)
                        em.ts(rn[:, 0:w], pn[:, 0:w], EPS, ALU.add)
                        em.act(rn[:, 0:w], rn[:, 0:w], AF.Sqrt)
                        em.op("dve", "reciprocal", out=rn[:, 0:w], in_=rn[:, 0:w])
                        if c < 16:
                            em.stt(nrm[:, b0:b0 + w], so[:, b0:b0 + w], 128 ** -0.5, rn[:, 0:w], ALU.mult, ALU.mult)
                        else:
                            em.tt(nrm[:, b0:b0 + w], so[:, b0:b0 + w], rn[:, 0:w], ALU.mult)
                    if c < 16:
                        em.dma(cx.qT[c, :, t0:t0 + L], nrm[:, 0:L])
                    else:
                        em.dma(cx.kT[c - 16, :, t0:t0 + L], nrm[:, 0:L])
                    src = nrm
                else:
                    src = so
                if c >= 16:
                    for b0 in range(0, L, 1024):
                        nt = min(8, (L - b0) // 128)
                        pt = em.ps()
                        ptb = pt[:].bitcast(BF16)
                        for k in range(nt):
                            em.mm(ptb[:, k * 128:(k + 1) * 128], src[:, b0 + k * 128:b0 + (k + 1) * 128], cx.identb[:], transpose=True)
                        tk = em.rot(es, "cv_tk", [128, 8, 128], BF16, 2)
                        em.copy(tk[:, 0:nt, :], ptb[:, 0:nt * 128].rearrange("p (t d) -> p t d", t=nt), e="act")
                        if c < 32:
                            dst = cx.ktok[t0 + b0:t0 + b0 + nt * 128, c - 16, :]
                        else:
                            dst = cx.vtok[t0 + b0:t0 + b0 + nt * 128, c - 32, :]
                        em.dma(dst.rearrange("(t p) d -> p t d", p=128), tk[:, 0:nt, :])


def phase_dn_scan(em, cx):
    seqs = [(i * LP, LP, i) for i in range(NPS)] + [(NPS * LP, LS, -1)]
    with em.scope() as es:
        st = dn_setup(em, es, cx.dncst, 32, False)
        for (t0, L, pi) in seqs:
            nch = L // 128
            for d in range(2):
                for h in range(32):
                    if pi >= 0:
                        em.memset(st.S[d][h][:], 0.0, e="pool")
                        em.memset(st.Sb[d][h][:], 0.0, e="pool")
                    else:
                        em.dma(st.S[d][h][:], cx.s0[d, h], stream="s0ld")
                        em.copy(st.Sb[d][h][:], st.S[d][h][:], e="act")
                order = range(nch) if d == 0 else range(nch - 1, -1, -1)
                for c in order:
                    a = t0 + c * 128
                    kT = em.rot(es, "s_kT", [128, 16, 128], BF16, 2)
                    em.dma(kT[:], cx.kT[:, :, a:a + 128].rearrange("h p t -> p h t"))
                    qT = em.rot(es, "s_qT", [128, 16, 128], BF16, 2)
                    em.dma(qT[:], cx.qT[:, :, a:a + 128].rearrange("h p t -> p h t"))
                    kt = em.rot(es, "s_kt", [128, 16, 128], BF16, 2)
                    em.dma(kt[:], cx.ktok[a:a + 128])
                    vt = em.rot(es, "s_vt", [128, 32, 128], BF16, 2)
                    em.dma(vt[:], cx.vtok[a:a + 128])
                    bgt = em.rot(es, "s_bg", [128, 128], F32, 2)
                    em.dma(bgt[:], cx.bg[a:a + 128, :])
                    oo = em.rot(es, "s_oo", [128, 32, 128], F32, 2)
                    dn_chunk(em, es, st, d, kT, qT, kt, vt, bgt[:, d * 32:(d + 1) * 32], bgt[:, 64 + d * 32:64 + (d + 1) * 32], 2, True, oo)
                    em.dma(cx.odn[d, a:a + 128, :], oo[:].rearrange("p h d -> p (h d)"))
                if pi >= 0:
                    for h in range(32):
                        em.dma(cx.nsd[pi, d, h], st.S[d][h][:], stream="nsdst")


def phase_post(em, cx, layer):
    groups = [(0, NPS * LP, 0)] + [(NPS * LP + i * 512, 512, 1) for i in range(LS // 512)]
    NCH = 32 if layer == 0 else 16
    w_o = cx.dn_w_o[0] if layer == 0 else cx.na_w_o[0]
    with em.scope() as es:
        keysT = peer_keysT(em, es, cx.peer_keys[layer], cx.identf)
        if layer == 0:
            gdn = em.sb("gdn", [128, 128], F32, es)
            em.dma(gdn[:], bcast_row(cx.dn_norm_g[0:1, :]))
        if layer == 1:
            fg = em.sb("fg", [128, D], F32, es)
            em.dma(fg[:], bcast_row(cx.final_g.rearrange("(o d) -> o d", o=1)))
        h2T = em.sb("h2T", [128, DC, 512], BF16, es)
        for (t0, n, var) in groups:
            ntp = n // 128
            with em.scope() as eg:
                x1 = [em.sb("x1", [128, D], F32, eg) for _ in range(ntp)]
                for tt in range(ntp):
                    em.dma(x1[tt][:], x_src(cx, layer, t0 + tt * 128))
                with em.scope() as eb:
                    g1 = load_mod(em, eb, cx, layer, var, 2, "g1")
                    ogT = em.sb("ogT", [128, NCH, 512], BF16, eb)
                    if layer == 0:
                        with em.scope() as ea:
                            for tt in range(ntp):
                                a = t0 + tt * 128
                                of = em.rot(ea, "of", [128, 32, 128], F32, 1)
                                ob = em.rot(ea, "ob", [128, 32, 128], F32, 1)
                                em.dma(of[:], cx.odn[0, a:a + 128, :].rearrange("p (h d) -> p h d", h=32))
                                em.dma(ob[:], cx.odn[1, a:a + 128, :].rearrange("p (h d) -> p h d", h=32))
                                zt = em.rot(ea, "zg", [128, 32, 128], BF16, 1)
                                em.dma(zt[:], cx.z[a:a + 128, :].rearrange("p (h d) -> p h d", h=32))
                                em.tt(of[:], of[:], ob[:], ALU.add, e="pool")
                                em.tt(ob[:], of[:], of[:], ALU.mult)
                                ms = em.rot(ea, "ms", [128, 32], F32, 2)
                                em.op("dve", "tensor_reduce", out=ms[:], in_=ob[:], op=ALU.add, axis=AX.X)
                                em.ts(ms[:], ms[:], 1.0 / 128, ALU.mult, EPS, ALU.add)
                                em.act(ms[:], ms[:], AF.Sqrt)
                                em.op("dve", "reciprocal", out=ms[:], in_=ms[:])
                                em.tt(of[:], of[:], ms[:].unsqueeze(2).to_broadcast([128, 32, 128]), ALU.mult)
                                em.tt(of[:], of[:], gdn[:].unsqueeze(1).to_broadcast([128, 32, 128]), ALU.mult, e="pool")
                                og = em.rot(ea, "og", [128, 32 * 128], BF16, 2)
                                em.tt(og[:].rearrange("p (h d) -> p h d", h=32), of[:], zt[:], ALU.mult)
                                for g0 in range(0, 32, 8):
                                    pt = em.ps()
                                    ptb = pt[:].bitcast(BF16)
                                    for k in range(8):
                                        em.mm(ptb[:, k * 128:(k + 1) * 128], og[:, (g0 + k) * 128:(g0 + k + 1) * 128], cx.identb[:], transpose=True)
                                    em.copy(ogT[:, g0:g0 + 8, tt * 128:(tt + 1) * 128], ptb[:].rearrange("p (c t) -> p c t", c=8), e="act")
                    else:
                        em.dma(ogT[:, :, 0:n], cx.oT[:, :, t0:t0 + n].rearrange("h p t -> p h t"))
                    for cc in range(4):
                        wo = em.rot(eb, "wo", [128, NCH, 512], BF16, 1)
                        for hf in range(2):
                            hs = slice(hf * NCH // 2, (hf + 1) * NCH // 2)
                            em.load_w(eb, wo[:, hs, :], w_o[hf * NCH * 64:(hf + 1) * NCH * 64, cc * 512:(cc + 1) * 512], NCH // 2, 512)
                        for tt in range(ntp):
                            po = em.ps()
                            for ch in range(NCH):
                                em.mm(po[:], ogT[:, ch, tt * 128:(tt + 1) * 128], wo[:, ch, :], start=(ch == 0), stop=(ch == NCH - 1))
                            tmp = em.rot(eb, "potmp", [128, 512], F32, 2)
                            em.tt(tmp[:], po[:], g1[:, cc * 512:(cc + 1) * 512], ALU.mult)
                            em.tt(x1[tt][:, cc * 512:(cc + 1) * 512], x1[tt][:, cc * 512:(cc + 1) * 512], tmp[:], ALU.add, e="pool")
                gm2, sh2 = make_gm(em, eg, cx, layer, var, 1, "n2")
                for tt in range(ntp):
                    norm_mod_T(em, eg, cx, x1[tt], gm2, sh2, h2T, tt * 128)
                    em.dma(cx.xmid[t0 + tt * 128:t0 + (tt + 1) * 128, :], x1[tt][:])
            with em.scope() as ep:
                acc = [em.sb("pacc", [128, D], F32, ep) for _ in range(ntp)]
                peer_pass(em, cx.nc, ep, DC, h2T, ntp, cx.peer_w_q[layer], keysT, cx.peer_u[layer], cx.peer_v[layer], cx.identb, acc)
                g2 = load_mod(em, ep, cx, layer, var, 5, "g2")
                for tt in range(ntp):
                    a = t0 + tt * 128
                    xr = em.rot(ep, "xr", [128, D], F32, 2)
                    em.dma(xr[:], cx.xmid[a:a + 128, :])
                    em.tt(acc[tt][:], acc[tt][:], g2[:], ALU.mult, e="pool")
                    em.tt(xr[:], xr[:], acc[tt][:], ALU.add)
                    if layer == 0:
                        em.dma(cx.xres[0][a:a + 128, :], xr[:])
                    else:
                        junk = em.rot(ep, "fjunk", [128, D], BF16, 1)
                        ss = em.rot(ep, "fss", [128, 1], F32, 2)
                        em.act(junk[:], xr[:], AF.Square, accum_out=ss[:])
                        em.ts(ss[:], ss[:], 1.0 / D, ALU.mult, EPS, ALU.add)
                        em.act(ss[:], ss[:], AF.Sqrt)
                        em.op("dve", "reciprocal", out=ss[:], in_=ss[:])
                        em.stt(acc[tt][:], xr[:], ss[:, 0:1], fg[:], ALU.mult, ALU.mult)
                        if a < NPS * LP:
                            em.dma(cx.yp[a:a + 128, :], acc[tt][:])
                        else:
                            em.dma(cx.ys[a - NPS * LP:a - NPS * LP + 128, :], acc[tt][:])


NA_SCALE = 128 ** -0.5


def attend(em, es, cx, chunks, qT_ap, nq, oT_dst, etab_ap=None):
    nchk = len(chunks)
    pS = em.ps()
    for i, (kt, v) in enumerate(chunks):
        em.mm(pS[:, i * nq:(i + 1) * nq], kt, qT_ap)
    E = em.rot(es, "at_E", [128, 512], BF16, 3)
    em.act(E[:, 0:nchk * nq], pS[:, 0:nchk * nq], AF.Exp, scale=NA_SCALE)
    if etab_ap is not None:
        nl = etab_ap.shape[1]
        v3 = E[:, 0:nl * nq].rearrange("p (m q) -> p m q", m=nl)
        em.tt(v3, v3, etab_ap, ALU.mult)
    pN = em.ps()
    for i, (kt, v) in enumerate(chunks):
        em.mm(pN[:, 0:nq], v, E[:, i * nq:(i + 1) * nq], start=(i == 0), stop=(i == nchk - 1))
    for i in range(nchk):
        em.mm(pN[:, 256:256 + nq], cx.onesb[:], E[:, i * nq:(i + 1) * nq], start=(i == 0), stop=(i == nchk - 1))
    rd = em.rot(es, "at_rd", [128, 256], F32, 3)
    em.op("dve", "reciprocal", out=rd[:, 0:nq], in_=pN[:, 256:256 + nq])
    em.tt(oT_dst, pN[:, 0:nq], rd[:, 0:nq], ALU.mult)


def phase_na(em, cx):
    groups = [(0, NPS * LP, 0)] + [(NPS * LP + i * 512, 512, 1) for i in range(LS // 512)]
    wq = cx.na_w_qkv[0]
    with em.scope() as es:
        hT = em.sb("hT", [128, DC, 512], BF16, es)
        for (t0, n, var) in groups:
            with em.scope() as eg:
                gm, sh = make_gm(em, eg, cx, 1, var, 0, "n1")
                for tt in range(n // 128):
                    xt = em.rot(eg, "xt", [128, D], F32, 2)
                    em.dma(xt[:], x_src(cx, 1, t0 + tt * 128))
                    norm_mod_T(em, eg, cx, xt, gm, sh, hT, tt * 128)
                for c in range(32):
                    if c % 2 == 0:
                        wb = em.rot(eg, "wqkb", [128, DC, 256], BF16, 2)
                        em.load_w(eg, wb, wq[:, c * 128:(c + 2) * 128], DC, 256)
                    pp = em.ps()
                    for dc in range(DC):
                        em.mm(pp[:, 0:n], wb[:, dc, (c % 2) * 128:(c % 2 + 1) * 128], hT[:, dc, 0:n], start=(dc == 0), stop=(dc == DC - 1))
                    pj = em.rot(eg, "pjn", [128, 512], BF16, 3)
                    em.copy(pj[:, 0:n], pp[:, 0:n], e="act")
                    dst = cx.qnT if c < 16 else cx.knT
                    em.dma(dst[c % 16, :, t0:t0 + n], pj[:, 0:n])
                for which in ([2, 1] if var == 0 else [2]):
                    for cc in range(4):
                        wv = em.rot(eg, "wvb", [128, DC, 512], BF16, 2)
                        em.load_w(eg, wv, wq[:, which * D + cc * 512:which * D + (cc + 1) * 512], DC, 512)
                        for tt in range(n // 128):
                            a = t0 + tt * 128
                            pv = em.ps()
                            for dc in range(DC):
                                em.mm(pv[:], hT[:, dc, tt * 128:(tt + 1) * 128], wv[:, dc, :], start=(dc == 0), stop=(dc == DC - 1))
                            if which == 2:
                                vb = em.rot(eg, "vnb", [128, 512], BF16, 3)
                                em.copy(vb[:], pv[:], e="act")
                                em.dma(cx.vn[a:a + 128, cc * 512:(cc + 1) * 512], vb[:])
                            if var == 0:
                                vf = em.rot(eg, "vnf", [128, 512], F32, 3)
                                em.ts(vf[:], pv[:], 1.0, ALU.mult)
                                dst = cx.ncv if which == 2 else cx.nck
                                em.dma(dst[a:a + 128, cc * 512:(cc + 1) * 512], vf[:])
    with em.scope() as es:
        for s in range(NPS):
            t0 = s * LP
            for h in range(16):
                kt = em.rot(es, "c_kt", [128, LP], BF16, 2)
                em.dma(kt[:], cx.knT[h, :, t0:t0 + LP])
                qt = em.rot(es, "c_qt", [128, LP], BF16, 2)
                em.dma(qt[:], cx.qnT[h, :, t0:t0 + LP])
                vv = em.rot(es, "c_v", [128, LP // 128, 128], BF16, 2)
                em.dma(vv[:], cx.vn[t0:t0 + LP, h * 128:(h + 1) * 128].rearrange("(t p) d -> p t d", p=128))
                ot = em.rot(es, "c_ot", [128, LP], BF16, 2)
                chunks = [(kt[:, kc * 128:(kc + 1) * 128], vv[:, kc, :]) for kc in range(LP // 128)]
                attend(em, es, cx, chunks, qt[:, :], LP, ot[:, :])
                em.dma(cx.oT[h, :, t0:t0 + LP], ot[:])
    with em.scope() as es:
        t0 = NPS * LP
        kcT = em.sb("kcT", [128, 16, 256], BF16, es)
        vc = em.sb("vc", [128, 2, 16, 128], BF16, es)
        for tt in range(2):
            ck = em.rot(es, "ckf", [128, 16, 128], F32, 2)
            em.dma(ck[:], cx.ck[tt * 128:(tt + 1) * 128])
            em.dma(vc[:, tt], cx.cv[tt * 128:(tt + 1) * 128], q="pool")
            for g0 in range(0, 16, 4):
                pt = em.ps()
                for k in range(4):
                    em.mm(pt[:, k * 128:(k + 1) * 128], ck[:, g0 + k, :], cx.identf[:], transpose=True)
                em.copy(kcT[:, g0:g0 + 4, tt * 128:(tt + 1) * 128], pt[:].rearrange("p (h t) -> p h t", h=4), e="act")
        okm = em.sb("okm", [128, 64], F32, es)
        em.dma(okm[:], cx.okm)
        for h in range(16):
            rp = em.rot(es, "rpx", [128, 14, 64], F32, 2)
            em.dma(rp[:], cx.rpbx[:, h])
            em.act(rp[:], rp[:], AF.Exp)
            etab = em.rot(es, "etab", [128, 14, 64], BF16, 2)
            em.tt(etab[:], rp[:], okm[:].unsqueeze(1).to_broadcast([128, 14, 64]), ALU.mult)
            kt = em.rot(es, "l_kt", [128, LS], BF16, 2)
            em.dma(kt[:], cx.knT[h, :, t0:t0 + LS])
            qt = em.rot(es, "l_qt", [128, LS], BF16, 2)
            em.dma(qt[:], cx.qnT[h, :, t0:t0 + LS])
            ve = em.rot(es, "l_ve", [128, 32, 128], BF16, 2)
            em.dma(ve[:], cx.vn[t0:t0 + LS, h * 128:(h + 1) * 128].rearrange("(t p) d -> p t d", p=128))
            vo = em.rot(es, "l_vo", [128, 31, 128], BF16, 2)
            em.dma(vo[:], cx.vn[t0 + 64:t0 + LS - 64, h * 128:(h + 1) * 128].rearrange("(t p) d -> p t d", p=128))
            ot = em.rot(es, "l_ot", [128, LS], BF16, 2)
            for r in range(64):
                r0 = min(max(r - 4, 0), 56)
                chunks = []
                for m in range(4):
                    a = (r0 + 2 * m) * 64
                    vsrc = ve[:, a // 128, :] if a % 128 == 0 else vo[:, (a - 64) // 128, :]
                    chunks.append((kt[:, a:a + 128], vsrc))
                for kc in range(2):
                    chunks.append((kcT[:, h, kc * 128:(kc + 1) * 128], vc[:, kc, h, :]))
                base = r0 - r + 7
                attend(em, es, cx, chunks, qt[:, r * 64:(r + 1) * 64], 64, ot[:, r * 64:(r + 1) * 64],
                       etab_ap=etab[:, base:base + 7:2, :])
            em.dma(cx.oT[h, :, t0:t0 + LS], ot[:])


_IN_SPECS = [
    ("xp", [NPS * LP, D], F32), ("xs", [LS, D], F32), ("cvec", [2, D], F32),
    ("s0", [2, 32, 128, 128], F32), ("ck", [256, 16, 128], F32), ("cv", [256, 16, 128], F32),
    ("ada_w", [2, D, 6 * D], F32), ("ada_b", [2, 6 * D], F32), ("norm1_g", [2, D], F32), ("norm2_g", [2, D], F32),
    ("final_g", [D], F32), ("dn_w_in", [1, D, 12416], F32), ("dn_conv_w", [1, 5, 8192], F32),
    ("dn_a_log", [1, 2, 32], F32), ("dn_dt_bias", [1, 2, 32], F32), ("dn_norm_g", [1, 128], F32),
    ("dn_w_o", [1, 4096, D], F32), ("na_w_qkv", [1, D, 3 * D], F32), ("na_w_o", [1, D, D], F32),
    ("peer_w_q", [2, D, 2048], F32), ("peer_keys", [2, 8, 2, 128, 128], F32),
    ("peer_u", [2, 16384, D], F32), ("peer_v", [2, 16384, D], F32),
    ("dncst", [128, 6, 128], F32), ("rpbx", [128, 16, 14, 64], F32), ("okm", [128, 64], F32),
]
_OUT_SPECS = [
    ("yp", [NPS * LP, D], F32), ("ys", [LS, D], F32), ("nsd", [NPS, 2, 32, 128, 128], F32),
    ("nck", [NPS * LP, D], F32), ("ncv", [NPS * LP, D], F32),
]


def build_nc(stop_after=99):
    nc = bass.Bass("TRN2", target_bir_lowering=False)
    cx = Ctx()
    cx.nc = nc
    for name, shape, dt in _IN_SPECS:
        setattr(cx, name, nc.dram_tensor(name, shape, dt, kind="ExternalInput").ap())
    for name, shape, dt in _OUT_SPECS:
        setattr(cx, name, nc.dram_tensor(name, shape, dt, kind="ExternalOutput").ap())
    cx.mod = dram(nc, "mod", [2, 2, 6 * D], F32)
    cx.projT = dram(nc, "projT", [64, 128, LTOT], F32)
    cx.z = dram(nc, "zs", [LTOT, 4096], BF16)
    cx.bg = dram(nc, "bg", [LTOT, 128], F32)
    cx.qT = dram(nc, "qT", [16, 128, LTOT], BF16)
    cx.kT = dram(nc, "kT", [16, 128, LTOT], BF16)
    cx.ktok = dram(nc, "ktok", [LTOT, 16, 128], BF16)
    cx.vtok = dram(nc, "vtok", [LTOT, 32, 128], BF16)
    cx.odn = dram(nc, "odn", [2, LTOT, 4096], F32)
    cx.xres = [dram(nc, "xres0", [LTOT, D], F32)]
    cx.xmid = dram(nc, "xmid", [LTOT, D], F32)
    cx.qnT = dram(nc, "qnT", [16, 128, LTOT], BF16)
    cx.knT = dram(nc, "knT", [16, 128, LTOT], BF16)
    cx.vn = dram(nc, "vn", [LTOT, D], BF16)
    cx.oT = dram(nc, "oT", [16, 128, LTOT], BF16)
    with contextlib.ExitStack() as es:
        em = Em(nc, es)
        em.init_psum()
        cx.identf = em.sb("identf", [128, 128], F32)
        em.dma(cx.identf[:], cx.dncst[:, 0, :])
        cx.identb = em.sb("identb", [128, 128], BF16)
        em.copy(cx.identb[:], cx.identf[:])
        cx.onesb = em.sb("onesb", [128, 128], BF16)
        em.memset(cx.onesb[:], 1.0)
        phase_adaln(em, cx)
        if stop_after >= 1:
            phase_dn_proj(em, cx)
            phase_dn_conv(em, cx)
            phase_dn_scan(em, cx)
        if stop_after >= 2:
            phase_post(em, cx, 0)
        if stop_after >= 3:
            phase_na(em, cx)
        if stop_after >= 4:
            phase_post(em, cx, 1)
        em.barrier()
        cx.n_inst = em.n_inst
    return nc, cx


def _rpb_layout(rpb):
    kc = np.arange(64)[:, None]
    qc = np.arange(64)[None, :]
    dc = np.clip(kc - qc, -15, 15) + 15
    out = np.empty((2, 64, 16, 14, 64), np.float32)
    for j in range(2):
        g = rpb[:, j:j + 14, :]
        out[j] = np.transpose(g[:, :, dc], (2, 0, 1, 3))
    return np.ascontiguousarray(out.reshape(128, 16, 14, 64))


def _ok_mask():
    qc = np.arange(64)
    c0 = np.clip(qc - 8, 0, 48)
    kc = np.arange(64)[:, None]
    ok = ((kc >= c0[None, :]) & (kc < c0[None, :] + 16)).astype(np.float32)
    return np.ascontiguousarray(np.concatenate([ok, ok], axis=0))


_NC_CACHE = {}


def kernel(x_prompt, x_sample, c, state_delta, cache_k, cache_v, c_ctx, ada_w, ada_b, norm1_g, norm2_g,
           final_g, dn_w_in, dn_conv_w, dn_a_log, dn_dt_bias, dn_norm_g, dn_w_o, na_w_qkv, na_rpb, na_w_o,
           peer_w_q, peer_keys, peer_u, peer_v):
    f = lambda a: np.ascontiguousarray(np.asarray(a, dtype=np.float32))
    x_prompt, x_sample, c, state_delta, cache_k, cache_v, c_ctx = map(f, (x_prompt, x_sample, c, state_delta, cache_k, cache_v, c_ctx))
    shared = dict(ada_w=f(ada_w), ada_b=f(ada_b), norm1_g=f(norm1_g), norm2_g=f(norm2_g), final_g=f(final_g),
                  dn_w_in=f(dn_w_in), dn_conv_w=f(dn_conv_w), dn_a_log=f(dn_a_log), dn_dt_bias=f(dn_dt_bias),
                  dn_norm_g=f(dn_norm_g), dn_w_o=f(dn_w_o), na_w_qkv=f(na_w_qkv), na_w_o=f(na_w_o),
                  peer_w_q=f(peer_w_q), peer_keys=f(peer_keys), peer_u=f(peer_u), peer_v=f(peer_v),
                  dncst=dn_host_consts(), rpbx=_rpb_layout(f(na_rpb)[0]), okm=_ok_mask())
    if "nc" not in _NC_CACHE:
        _NC_CACHE["nc"] = build_nc()
    nc, cx = _NC_CACHE["nc"]
    in_maps = []
    for core in range(8):
        b = core % 2
        m = dict(shared)
        m["xp"] = np.ascontiguousarray(x_prompt[core * NPS:(core + 1) * NPS].reshape(NPS * LP, D))
        m["xs"] = x_sample[b]
        m["cvec"] = np.ascontiguousarray(np.stack([c_ctx, c[b]], axis=0))
        m["s0"] = np.ascontiguousarray(state_delta[b, 0])
        m["ck"] = np.ascontiguousarray(cache_k[b, 0])
        m["cv"] = np.ascontiguousarray(cache_v[b, 0])
        in_maps.append(m)
    res = run_bass_kernel_spmd(nc, in_maps, core_ids=list(range(8)))
    r = res.results
    y_prompt = np.concatenate([r[i]["yp"].reshape(NPS, LP, D) for i in range(8)], axis=0)
    y_sample = np.stack([r[0]["ys"], r[1]["ys"]], axis=0)
    nsd = np.concatenate([r[i]["nsd"] for i in range(8)], axis=0)[:, None]
    nck = np.concatenate([r[i]["nck"].reshape(NPS, LP, 16, 128) for i in range(8)], axis=0)[:, None]
    ncv = np.concatenate([r[i]["ncv"].reshape(NPS, LP, 16, 128) for i in range(8)], axis=0)[:, None]
    return (y_prompt.astype(np.float32), y_sample.astype(np.float32), nsd.astype(np.float32),
            nck.astype(np.float32), ncv.astype(np.float32))
```

```python
from concourse.bass_utils import run_bass_kernel_spmd
import contextlib
import numpy as np
import concourse.bass as bass
import concourse.mybir as mybir

F32 = mybir.dt.float32
BF16 = mybir.dt.bfloat16
I32 = mybir.dt.int32
AF = mybir.ActivationFunctionType
ALU = mybir.AluOpType
AX = mybir.AxisListType


class Em:
    ENG = ("pe", "dve", "act", "pool", "sp")

    def __init__(self, nc, es):
        self.nc = nc
        self.es = es
        self.eng = {"pe": nc.tensor, "dve": nc.vector, "act": nc.scalar, "pool": nc.gpsimd, "sp": nc.sync}
        self.sem = {e: es.enter_context(nc.semaphore("s_" + e)) for e in self.ENG if e != "sp"}
        self.cnt = {e: 0 for e in self.ENG}
        self.seen = {e: {} for e in self.ENG}
        self.last_w = {}
        self.readers = {}
        self.dsem = {}
        self.dfree = []
        self.dscopes = [set()]
        self.n_inst = 0
        self.psum = []
        self.ps_i = 0
        self.uid = 0

    def sb(self, name, shape, dt=F32, es=None):
        self.uid += 1
        return (es or self.es).enter_context(self.nc.sbuf_tensor(f"{name}_{self.uid}", list(shape), dt))

    def init_psum(self):
        for i in range(8):
            self.psum.append(self.es.enter_context(self.nc.psum_tensor(f"ps{i}", [128, 512], F32)))

    def ps(self):
        t = self.psum[self.ps_i % 8]
        self.ps_i += 1
        return t

    @staticmethod
    def key(ap):
        if isinstance(ap, str):
            return ap
        return ap.tensor.name

    def _deps(self, e, reads, writes):
        need = {}
        def add(tok):
            k, v = tok
            if k == "pe" and e == "pe":
                return
            if need.get(k, 0) < v:
                need[k] = v
        for r in reads:
            t = self.last_w.get(r)
            if t:
                add(t)
            if r.startswith("ps"):
                for t in self.readers.get(r, ()):
                    if t[0] != e:
                        add(t)
        for w in writes:
            t = self.last_w.get(w)
            if t:
                add(t)
            for t in self.readers.get(w, ()):
                add(t)
        for k, v in need.items():
            if k.startswith("dma:"):
                v = self.dsem[k[4:]][1]
            if self.seen[e].get(k, 0) < v:
                sem = self.dsem[k[4:]][0] if k.startswith("dma:") else self.sem[k]
                self.eng[e].wait_ge(sem, v)
                self.seen[e][k] = v
                self.n_inst += 1

    def _commit(self, tok, reads, writes):
        for w in writes:
            self.last_w[w] = tok
            self.readers[w] = set()
        for r in reads:
            if r not in writes:
                self.readers.setdefault(r, set()).add(tok)

    def _rw(self, kw, extra_r=(), extra_w=()):
        reads, writes = [], []
        for k, v in kw.items():
            if isinstance(v, bass.AP):
                if k in ("out", "accum_out"):
                    writes.append(self.key(v))
                else:
                    reads.append(self.key(v))
        reads += [self.key(x) for x in extra_r]
        writes += [self.key(x) for x in extra_w]
        return reads, writes

    def op(self, e, name, extra_r=(), extra_w=(), **kw):
        reads, writes = self._rw(kw, extra_r, extra_w)
        self._deps(e, reads, writes)
        inst = getattr(self.eng[e], name)(**kw)
        self.cnt[e] += 1
        inst.then_inc(self.sem[e], 1)
        self.n_inst += 1
        self._commit((e, self.cnt[e]), reads, writes)
        return inst

    def mm(self, out, lhsT, rhs, start=True, stop=True, transpose=False, **kw):
        reads = [self.key(lhsT), self.key(rhs)]
        writes = [self.key(out)]
        self._deps("pe", reads, writes)
        if transpose:
            inst = self.nc.tensor.transpose(out, lhsT, rhs, **kw)
        else:
            inst = self.nc.tensor.matmul(out, lhsT=lhsT, rhs=rhs, start=start, stop=stop, **kw)
        self.n_inst += 1
        if stop:
            self.cnt["pe"] += 1
            inst.then_inc(self.sem["pe"], 1)
            tok = ("pe", self.cnt["pe"])
        else:
            tok = ("pe", self.cnt["pe"] + 1)
        self._commit(tok, reads, writes)
        return inst

    def dma(self, out, in_, stream=None, q="sp", **kw):
        reads = [self.key(in_)]
        writes = [self.key(out)]
        if stream is None:
            stream = self.key(out) if "DRam" not in type(out.tensor).__name__ else self.key(in_)
        if stream not in self.dsem:
            self._new_stream(stream)
        self._deps(q, reads, writes)
        inst = self.eng[q].dma_start(out=out, in_=in_, **kw)
        self.dsem[stream][1] += 16
        inst.then_inc(self.dsem[stream][0], 16)
        self.n_inst += 1
        self._commit(("dma:" + stream, self.dsem[stream][1]), reads, writes)
        return inst

    def _new_stream(self, stream):
        if self.dfree:
            self.dsem[stream] = self.dfree.pop()
        else:
            self.dsem[stream] = [self.es.enter_context(self.nc.semaphore("d%d" % len(self.dsem))), 0]
        self.dscopes[-1].add(stream)

    def barrier(self):
        for e in self.ENG:
            for p in self.sem:
                v = self.cnt[p]
                if v and self.seen[e].get(p, 0) < v:
                    self.eng[e].wait_ge(self.sem[p], v)
                    self.seen[e][p] = v
            for s, (sem, v) in self.dsem.items():
                k = "dma:" + s
                if v and self.seen[e].get(k, 0) < v:
                    self.eng[e].wait_ge(sem, v)
                    self.seen[e][k] = v
        self.last_w.clear()
        self.readers.clear()

    def act(self, out, in_, func, e="act", **kw):
        return self.op(e, "activation", out=out, in_=in_, func=func, **kw)

    def tt(self, out, in0, in1, op, e="dve"):
        return self.op(e, "tensor_tensor", out=out, in0=in0, in1=in1, op=op)

    def ts(self, out, in0, s1, op0, s2=None, op1=None, e="dve", **kw):
        if op1 is None:
            return self.op(e, "tensor_scalar", out=out, in0=in0, scalar1=s1, scalar2=None, op0=op0, **kw)
        return self.op(e, "tensor_scalar", out=out, in0=in0, scalar1=s1, scalar2=s2, op0=op0, op1=op1, **kw)

    def stt(self, out, in0, scalar, in1, op0, op1, e="dve"):
        return self.op(e, "scalar_tensor_tensor", out=out, in0=in0, scalar=scalar, in1=in1, op0=op0, op1=op1)

    def copy(self, out, in_, e="dve"):
        if e == "act":
            return self.op("act", "activation", out=out, in_=in_, func=AF.Copy)
        return self.op(e, "tensor_copy", out=out, in_=in_)

    def memset(self, ap, val, e="dve"):
        reads, writes = [], [self.key(ap)]
        self._deps(e, reads, writes)
        inst = self.eng[e].memset(ap, val)
        self.cnt[e] += 1
        inst.then_inc(self.sem[e], 1)
        self.n_inst += 1
        self._commit((e, self.cnt[e]), reads, writes)
        return inst


def _dbg(self, name, ap, dt=None):
    shape = list(ap.shape)
    d = self.nc.dram_tensor("dbg_" + name, shape, dt or ap.dtype, kind="ExternalOutput").ap()
    self.dma(d, ap)
Em.dbg = _dbg


@contextlib.contextmanager
def _scope(self):
    with contextlib.ExitStack() as es:
        self.dscopes.append(set())
        yield es
        self.barrier()
        for stream in self.dscopes.pop():
            ent = self.dsem.pop(stream)
            self.dfree.append(ent)
            for e in self.ENG:
                self.seen[e].pop("dma:" + stream, None)
Em.scope = _scope


def _rot(self, es, name, shape, dt=F32, n=2):
    pools = getattr(es, "_pools", None)
    if pools is None:
        pools = {}
        es._pools = pools
    ent = pools.get(name)
    if ent is None:
        ent = [[self.sb(name, shape, dt, es) for _ in range(n)], 0]
        pools[name] = ent
    t = ent[0][ent[1] % len(ent[0])]
    ent[1] += 1
    return t
Em.rot = _rot


def _allgather(self, out, in_, groups, stream="cc"):
    reads = [self.key(in_)]
    writes = [self.key(out)]
    if stream not in self.dsem:
        self._new_stream(stream)
    self._deps("pool", reads, writes)
    inst = self.nc.gpsimd.collective_compute("AllGather", op=ALU.bypass, replica_groups=groups, ins=[in_], outs=[out])
    self.dsem[stream][1] += 16
    inst.then_inc(self.dsem[stream][0], 16)
    self.n_inst += 1
    self._commit(("dma:" + stream, self.dsem[stream][1]), reads, writes)
    return inst
Em.allgather = _allgather


def _load_w(self, es, dst, src, kc, ncols, piece=256):
    if not isinstance(dst, bass.AP):
        dst = dst[:]
    self.dma(dst, src.rearrange("(c p) n -> p c n", p=128), q="pool")
Em.load_w = _load_w


def peer_keysT(em, es, keys_l, ident_f):
    kf = em.sb("keys_f", [128, 16, 128], F32, es)
    em.dma(kf[:], keys_l.rearrange("h p n d -> n (h p) d"))
    keysT = em.sb("keysT", [128, 16, 128], BF16, es)
    for g in range(4):
        pt = em.ps()
        for k in range(4):
            c = g * 4 + k
            em.mm(pt[:, k * 128:(k + 1) * 128], kf[:, c, :], ident_f[:], transpose=True)
        em.copy(keysT[:, g * 4:(g + 1) * 4, :], pt[:].rearrange("p (c n) -> p c n", c=4), e="act")
    return keysT


def peer_pass(em, nc, es0, DC, hT2, ntp, wq_l, keysT, u_l, v_l, ident_b, acc_out, NI=128, IB=2, dbg=False):
    D = DC * 128
    NT = ntp * 128
    with em.scope() as es:
        stok = [em.sb("stok", [128, 8, 2, 128], F32, es) for _ in range(ntp)]
        diag = [em.sb("diag", [128, 8, 128], BF16, es) for _ in range(ntp)]
        _cm = em.scope()
        es_q = _cm.__enter__()
        qT = em.sb("qT", [128, 16, NT], BF16, es_q)
        wqb = [em.sb("wqb", [128, DC, 256], BF16, es_q) for _ in range(2)]
        for c in range(16):
            wb = wqb[(c // 2) % 2]
            if c % 2 == 0:
                em.load_w(es_q, wb, wq_l[:, c * 128:(c + 2) * 128], DC, 256)
            pq = em.ps()
            for dc in range(DC):
                em.mm(pq[:, 0:NT], wb[:, dc, (c % 2) * 128:(c % 2 + 1) * 128], hT2[:, dc, :], start=(dc == 0), stop=(dc == DC - 1))
            em.copy(qT[:, c, :], pq[:, 0:NT], e="act")
        with em.scope() as es2:
            top = em.sb("top", [128, 2, 16], F32, es2)
            work = em.sb("work", [128, 128], F32, es2)
            cand = em.sb("cand", [128, 16, 16], F32, es2)
            cand2 = em.sb("cand2", [128, 256], F32, es2)
            c24 = em.sb("c24", [128, 24], F32, es2)
            tau = em.sb("tau", [128, 8], F32, es2)
            zs = em.sb("zs", [128, 8], F32, es2)
            ejunk = em.sb("ejunk", [128, 16], F32, es2)
            ntau = em.sb("ntau", [128, 1], F32, es2)
            for tt in range(ntp):
                st = stok[tt]
                for g in range(4):
                    pt = em.ps()
                    for k in range(4):
                        c = g * 4 + k
                        em.mm(pt[:, k * 128:(k + 1) * 128], qT[:, c, tt * 128:(tt + 1) * 128], keysT[:, c, :])
                    em.copy(st[:, 2 * g:2 * g + 2, :, :], pt[:].rearrange("p (h q n) -> p h q n", h=2, q=2), e="act")
                for h in range(8):
                    for p in range(2):
                        src = st[:, h, p, :]
                        em.op("dve", "max", out=top[:, p, 0:8], in_=src)
                        em.op("dve", "match_replace", out=work[:], in_to_replace=top[:, p, 0:8], in_values=src, imm_value=-1e30)
                        em.op("dve", "max", out=top[:, p, 8:16], in_=work[:])
                    em.tt(cand[:], top[:, 0, :].unsqueeze(2).to_broadcast([128, 16, 16]),
                          top[:, 1, :].unsqueeze(1).to_broadcast([128, 16, 16]), ALU.add)
                    cf = cand[:].rearrange("p a b -> p (a b)")
                    em.op("dve", "max", out=c24[:, 0:8], in_=cf)
                    em.op("dve", "match_replace", out=cand2[:], in_to_replace=c24[:, 0:8], in_values=cf, imm_value=-1e30)
                    em.op("dve", "max", out=c24[:, 8:16], in_=cand2[:])
                    em.op("dve", "match_replace", out=cand2[:], in_to_replace=c24[:, 8:16], in_values=cand2[:], imm_value=-1e30)
                    em.op("dve", "max", out=c24[:, 16:24], in_=cand2[:])
                    em.ts(tau[:, h:h + 1], c24[:, 15:16], c24[:, 16:17], ALU.add, 0.5, ALU.mult)
                    em.ts(ntau[:], tau[:, h:h + 1], -1.0, ALU.mult)
                    em.act(ejunk[:], c24[:, 0:16], AF.Exp, bias=ntau[:, 0:1], accum_out=zs[:, h:h + 1])
                em.tt(st[:, :, 0, :], st[:, :, 0, :], tau[:].unsqueeze(2).to_broadcast([128, 8, 128]), ALU.subtract)
                em.op("dve", "reciprocal", out=zs[:], in_=zs[:])
                for h in range(8):
                    em.ts(diag[tt][:, h, :], ident_b[:], zs[:, h:h + 1], ALU.mult)
                if dbg and tt == 0:
                    em.dbg("tau", tau[:]); em.dbg("zs", zs[:]); em.dbg("stok", st[:]); em.dbg("c24", c24[:]); em.dbg("top", top[:])
                    em.dbg("qT", qT[:])
        _cm.__exit__(None, None, None)
        urow = [em.sb("urow", [128, D], BF16, es) for _ in range(3)]
        uT = [em.sb("uT", [128, DC, 128], BF16, es) for _ in range(2)]
        vblk = [[em.sb("vblk", [128, D], BF16, es) for _ in range(IB)] for _ in range(3)]
        gS = [em.sb("gS", [128, NT], BF16, es) for _ in range(2)]
        AT = [[em.sb("AT", [128, NT], BF16, es) for _ in range(IB)] for _ in range(2)]
        Pp = [em.sb("Pp", [128, 8, 128], F32, es) for _ in range(2)]
        Ee = [em.sb("Ee", [128, 8, 128], BF16, es) for _ in range(2)]
        Gg = [em.sb("Gg", [128, 8, 128], BF16, es) for _ in range(2)]
        Mk = [em.sb("Mk", [128, 8, 128], BF16, es) for _ in range(2)]
        for a in acc_out:
            em.memset(a[:], 0.0, e="pool")
        nblk = NI // IB
        k2 = 0

        def load(i):
            b, ii = divmod(i, IB)
            em.dma(urow[i % 3][:], u_l[i * 128:(i + 1) * 128, :], q="pool")
            em.dma(vblk[b % 3][ii][:], v_l[i * 128:(i + 1) * 128, :], q="pool")

        load(0)
        load(1)

        pS_of = {}

        def front0(i):
            ur = urow[i % 3]
            ut = uT[i % 2]
            for g0 in range(0, DC, 8):
                n = min(8, DC - g0)
                pt = em.ps()
                ptb = pt[:].bitcast(BF16)
                for k in range(n):
                    em.mm(ptb[:, k * 128:(k + 1) * 128], ur[:, (g0 + k) * 128:(g0 + k + 1) * 128], ident_b[:], transpose=True)
                em.copy(ut[:, g0:g0 + n, :], ptb[:, 0:n * 128].rearrange("p (c e) -> p c e", c=n), e="act")
            if i + 2 < NI:
                load(i + 2)

        def front1(i):
            ut = uT[i % 2]
            pS = em.ps()
            for dc in range(DC):
                em.mm(pS[:, 0:NT], ut[:, dc, :], hT2[:, dc, :], start=(dc == 0), stop=(dc == DC - 1))
            pS_of[i] = pS

        def front2(i):
            em.act(gS[i % 2][:], pS_of.pop(i)[:, 0:NT], AF.Gelu)

        kk = [0]

        def back(i):
            b, ii = divmod(i, IB)
            pG = em.ps()
            nxt = i + 1 if i + 1 < NI else None
            for tt in range(ntp):
                st = stok[tt]
                k2 = kk[0]
                kk[0] += 1
                pp, ee, gg = Pp[k2 % 2], Ee[k2 % 2], Gg[k2 % 2]
                em.tt(pp[:], st[:, :, 1, :], st[:, :, 0, i:i + 1].to_broadcast([128, 8, 128]), ALU.add, e="pool")
                em.act(ee[:], pp[:], AF.Exp)
                em.stt(gg[:], pp[:], 0.0, ee[:], ALU.is_ge, ALU.mult)
                for h in range(8):
                    em.mm(pG[:, tt * 128:(tt + 1) * 128], gg[:, h, :], diag[tt][:, h, :], start=(h == 0), stop=(h == 7))
                if nxt is not None:
                    if tt == 0:
                        front0(nxt)
                    elif tt == min(1, ntp - 1):
                        front1(nxt)
            if nxt is not None:
                if ntp == 1:
                    front1(nxt)
                front2(nxt)
            em.tt(AT[b % 2][ii][:], pG[:, 0:NT], gS[i % 2][:], ALU.mult)

        def accum(b):
            vb = vblk[b % 3]
            at = AT[b % 2]
            for tt in range(ntp):
                banks = [em.ps() for _ in range((D + 511) // 512)]
                for cc, bk in enumerate(banks):
                    w = min(512, D - cc * 512)
                    for ii in range(IB):
                        em.mm(bk[:, 0:w], at[ii][:, tt * 128:(tt + 1) * 128], vb[ii][:, cc * 512:cc * 512 + w],
                              start=(ii == 0), stop=(ii == IB - 1))
                for cc, bk in enumerate(banks):
                    w = min(512, D - cc * 512)
                    em.tt(acc_out[tt][:, cc * 512:cc * 512 + w], acc_out[tt][:, cc * 512:cc * 512 + w], bk[:, 0:w], ALU.add)

        front0(0)
        front1(0)
        front2(0)
        for i in range(NI):
            back(i)
            if i % IB == 0 and i >= IB:
                accum(i // IB - 1)
        accum(NI // IB - 1)

NEG = -30000.0


def dn_host_consts():
    p = np.arange(128)[:, None]
    f = np.arange(128)[None, :]
    c = np.zeros((128, 6, 128), np.float32)
    c[:, 0] = (p == f)
    c[:, 1] = (p <= f)
    c[:, 2] = (p >= f)
    c[:, 3] = np.where(p > f, 0.0, NEG)
    c[:, 4] = np.where(p < f, 0.0, NEG)
    c[:, 5] = 1.0
    return c


class DnState:
    pass


def dn_setup(em, es, consts_d, NH, aug):
    st = DnState()
    st.NH = NH
    st.W = 256 if aug else 128
    st.cst = em.sb("dncst", [128, 6, 128], F32, es)
    em.dma(st.cst[:], consts_d)
    st.identb = em.sb("dnidb", [128, 128], BF16, es)
    em.copy(st.identb[:], st.cst[:, 0, :])
    st.negT = em.sb("dnnegT", [128, 2, 128], F32, es)
    em.ts(st.negT[:], st.cst[:, 1:3, :], -1.0, ALU.add, -NEG, ALU.mult)
    st.S = [[em.sb("S", [128, st.W], F32, es) for _ in range(NH)] for _ in range(2)]
    st.Sb = [[em.sb("Sb", [128, st.W], BF16, es) for _ in range(NH)] for _ in range(2)]
    return st


def dn_chunk(em, es, st, d, kT, qT, ktok, vtok, beta, g, rep, want_o, o_out, aug=False, dbg=None, stage=99):
    NH = st.NH
    W = st.W
    ident = st.cst[:, 0, :]
    ones = st.cst[:, 5, :]
    Mincl = st.cst[:, 1 + d, :]
    negS = st.cst[:, 3 + d, :]
    negT = st.negT[:, d, :]
    pc = em.ps()
    em.mm(pc[:, 0:NH], Mincl, g[:, :])
    em.mm(pc[:, 128:128 + NH], ones, g[:, :])
    gc = em.rot(es, "gc", [128, 6, NH], F32, 2)
    em.copy(gc[:, 0, :], pc[:, 0:NH])
    em.ts(gc[:, 1, :], pc[:, 0:NH], -1.0, ALU.mult)
    em.act(gc[:, 2, :], pc[:, 0:NH], AF.Exp)
    em.tt(gc[:, 2, :], gc[:, 2, :], beta[:, :], ALU.mult)
    em.tt(gc[:, 5, :], pc[:, 128:128 + NH], gc[:, 0, :], ALU.subtract)
    em.act(gc[:, 3, :], gc[:, 5, :], AF.Exp)
    em.act(gc[:, 4, :], pc[:, 128:128 + NH], AF.Exp)
    if stage < 1:
        return
    nhk = NH // rep
    for hk in range(nhk):
        pk = em.ps()
        em.mm(pk[:, 0:128], kT[:, hk, :], kT[:, hk, :])
        em.mm(pk[:, 128:256], kT[:, hk, :], qT[:, hk, :])
        kkq = em.rot(es, "kkq", [128, 256], F32, 2)
        em.copy(kkq[:], pk[:, 0:256], e="act")
        for r in range(rep):
            h = hk * rep + r
            S, Sb = st.S[d][h], st.Sb[d][h]
            gM = em.rot(es, "gM", [128, 128], F32, 2)
            em.ts(gM[:], Mincl, g[:, h:h + 1], ALU.mult)
            pg = em.ps()
            em.mm(pg[:, 0:128], ones, gM[:])
            t1 = em.rot(es, "t1", [128, 128], F32, 2)
            em.stt(t1[:], pg[:, 0:128], -1.0, negS, ALU.mult, ALU.add)
            em.act(t1[:], t1[:], AF.Exp, bias=gc[:, 0, h:h + 1])
            A = em.rot(es, "A", [128, 128], F32, 2)
            em.stt(A[:], t1[:], beta[:, h:h + 1], kkq[:, 0:128], ALU.mult, ALU.mult)
            t2 = em.rot(es, "t2", [128, 128], F32, 2)
            em.tt(t2[:], pg[:, 0:128], negT, ALU.add)
            em.act(t2[:], t2[:], AF.Exp, bias=gc[:, 1, h:h + 1])
            if want_o:
                qkT = em.rot(es, "qkT", [128, 128], BF16, 2)
                em.tt(qkT[:], t2[:], kkq[:, 128:256], ALU.mult)
                eg = em.rot(es, "eg", [128, 128], F32, 2)
                em.act(eg[:], pg[:, 0:128], AF.Exp)
                qgT = em.rot(es, "qgT", [128, 128], BF16, 2)
                em.tt(qgT[:], qT[:, hk, :], eg[:], ALU.mult)
            if stage < 2:
                continue
            pa = em.ps()
            em.mm(pa[:, 0:128], A[:], ident, transpose=True)
            B = em.rot(es, "B", [128, 128], F32, 3)
            BT = em.rot(es, "BT", [128, 128], F32, 3)
            P = em.rot(es, "P", [128, 128], F32, 3)
            em.ts(B[:], A[:], -1.0, ALU.mult)
            em.ts(BT[:], pa[:, 0:128], -1.0, ALU.mult)
            em.tt(P[:], BT[:], ident, ALU.add)
            if stage < 2.2:
                continue
            for lvl in range(1, 7 if stage >= 2.6 else 2):
                last = lvl == 6
                if stage < 2.4 and lvl >= 1:
                    pb = em.ps()
                    em.mm(pb[:, 0:128], BT[:], B[:])
                    continue
                pb = em.ps()
                em.mm(pb[:, 0:128], BT[:], B[:])
                if not last:
                    em.mm(pb[:, 128:256], B[:], BT[:])
                if stage < 2.42:
                    continue
                B2 = em.rot(es, "B", [128, 128], F32, 3)
                em.copy(B2[:], pb[:, 0:128], e="act")
                if stage < 2.44:
                    B = B2
                    continue
                if not last:
                    BT2 = em.rot(es, "BT", [128, 128], F32, 3)
                    em.ts(BT2[:], pb[:, 128:256], 1.0, ALU.mult) if "dve" == "dve" else em.copy(BT2[:], pb[:, 128:256], e="act")
                    BT = BT2
                B = B2
                if stage < 2.5:
                    continue
                pp = em.ps()
                em.mm(pp[:, 0:128], B[:], P[:])
                P2 = em.rot(es, "P", [128, 128], F32, 3)
                em.tt(P2[:], P[:], pp[:, 0:128], ALU.add)
                P = P2
            if stage < 3:
                continue
            Pb = em.rot(es, "Pb", [128, 128], BF16, 2)
            em.copy(Pb[:], P[:], e="act")
            bv = em.rot(es, "bv", [128, 128], BF16, 2)
            em.ts(bv[:], vtok[:, h, :], beta[:, h:h + 1], ALU.mult)
            bk = em.rot(es, "bk", [128, 128], BF16, 2)
            em.ts(bk[:], ktok[:, hk, :], gc[:, 2, h:h + 1], ALU.mult)
            kd = em.rot(es, "kd", [128, 128], BF16, 2)
            em.ts(kd[:], ktok[:, hk, :], gc[:, 3, h:h + 1], ALU.mult)
            pw = em.ps()
            em.mm(pw[:, 0:128], bk[:], Pb[:])
            nwkT = em.rot(es, "nwkT", [128, 128], BF16, 2)
            em.ts(nwkT[:], pw[:, 0:128], -1.0, ALU.mult)
            pwv = em.ps()
            em.mm(pwv[:, 0:128], Pb[:], bv[:], start=True, stop=False)
            em.mm(pwv[:, 0:128], nwkT[:], Sb[:, 0:128], start=False, stop=True)
            if aug:
                em.mm(pwv[:, 128:256], nwkT[:], Sb[:, 128:256], start=True, stop=True)
            wb = em.rot(es, "wb", [128, W], BF16, 2)
            em.copy(wb[:], pwv[:, 0:W], e="act")
            if want_o:
                po = em.ps()
                em.mm(po[:, 0:128], qgT[:], Sb[:, 0:128], start=True, stop=False)
                em.mm(po[:, 0:128], qkT[:], wb[:, 0:128], start=False, stop=True)
                em.copy(o_out[:, h, :], po[:, 0:128], e="act")
            pS = em.ps()
            em.mm(pS[:, 0:W], kd[:], wb[:])
            em.stt(S[:], S[:], gc[:, 4, h:h + 1], pS[:, 0:W], ALU.mult, ALU.add)
            em.copy(Sb[:], S[:], e="act")
            if dbg is not None and h == 0:
                dbg(dict(A=A, P=P, t2=t2, wb=wb, kkq=kkq, gc=gc))


D = 2048
DC = 16
LP = 256
NPS = 2
LS = 4096
LTOT = NPS * LP + LS
DN_QKV = 8192
EPS = 1e-6


def dram(nc, name, shape, dt):
    return nc.dram_tensor(name, list(shape), dt, kind="Internal").ap()


class Ctx:
    pass


def bcast_row(ap_row, n=128):
    a = ap_row.partition_broadcast(n)
    if len(a.shape) == 3:
        a = a.rearrange("p o d -> p (o d)")
    return a


def load_mod(em, es, cx, layer, variant, k, name):
    t = em.rot(es, name, [128, D], F32, 1)
    em.dma(t[:], bcast_row(cx.mod[layer, variant:variant + 1, k * D:(k + 1) * D]))
    return t


def make_gm(em, es, cx, layer, variant, which, name):
    sh = load_mod(em, es, cx, layer, variant, 3 * which + 0, name + "sh")
    sc = load_mod(em, es, cx, layer, variant, 3 * which + 1, name + "sc")
    g = em.rot(es, name + "g", [128, D], F32, 1)
    gsrc = (cx.norm1_g if which == 0 else cx.norm2_g)[layer:layer + 1, :]
    em.dma(g[:], bcast_row(gsrc))
    em.stt(sc[:], sc[:], 1.0, g[:], ALU.add, ALU.mult)
    return sc, sh


def norm_mod_T(em, es, cx, xt, gm, sh, hT, col0):
    junk = em.rot(es, "nm_junk", [128, D], BF16, 1)
    ss = em.rot(es, "nm_ss", [128, 1], F32, 2)
    em.act(junk[:], xt[:], AF.Square, accum_out=ss[:])
    rs = em.rot(es, "nm_rs", [128, 1], F32, 2)
    em.ts(rs[:], ss[:], 1.0 / D, ALU.mult, EPS, ALU.add)
    em.act(rs[:], rs[:], AF.Sqrt)
    em.op("dve", "reciprocal", out=rs[:], in_=rs[:])
    t = em.rot(es, "nm_t", [128, D], F32, 1)
    em.stt(t[:], xt[:], rs[:, 0:1], gm[:], ALU.mult, ALU.mult)
    hb = em.rot(es, "nm_hb", [128, D], BF16, 2)
    em.tt(hb[:], t[:], sh[:], ALU.add, e="pool")
    for g0 in range(0, DC, 8):
        pt = em.ps()
        ptb = pt[:].bitcast(BF16)
        for k in range(8):
            em.mm(ptb[:, k * 128:(k + 1) * 128], hb[:, (g0 + k) * 128:(g0 + k + 1) * 128], cx.identb[:], transpose=True)
        em.copy(hT[:, g0:g0 + 8, col0:col0 + 128], ptb[:].rearrange("p (c t) -> p c t", c=8), e="act")


def x_src(cx, layer_in, tok0):
    if layer_in == 0:
        if tok0 < NPS * LP:
            return cx.xp[tok0:tok0 + 128, :]
        return cx.xs[tok0 - NPS * LP:tok0 - NPS * LP + 128, :]
    return cx.xres[layer_in - 1][tok0:tok0 + 128, :]


def phase_adaln(em, cx):
    nc = cx.nc
    with em.scope() as es:
        cv = em.sb("cv", [128, DC, 2], F32, es)
        with cx.nc.allow_non_contiguous_dma("tiny transposed load of conditioning vectors"):
            for v in range(2):
                em.dma(cv[:, :, v], cx.cvec[v, :].rearrange("(c p) -> p c", p=128))
        em.act(cv[:], cv[:], AF.Silu)
        for l in range(2):
            for cc in range(24):
                w = em.rot(es, "adaw", [128, DC, 512], F32, 2)
                em.dma(w[:], cx.ada_w[l, :, cc * 512:(cc + 1) * 512].rearrange("(c p) n -> p c n", p=128))
                b = em.rot(es, "adab", [2, 512], F32, 2)
                em.dma(b[:], bcast_row(cx.ada_b[l:l + 1, cc * 512:(cc + 1) * 512], 2))
                pm = em.ps()
                for dc in range(DC):
                    em.mm(pm[0:2, :], cv[:, dc, :], w[:, dc, :], start=(dc == 0), stop=(dc == DC - 1))
                m = em.rot(es, "adam", [2, 512], F32, 2)
                em.tt(m[:], pm[0:2, :], b[:], ALU.add)
                em.dma(cx.mod[l, :, cc * 512:(cc + 1) * 512], m[:])


def phase_dn_proj(em, cx):
    groups = [(0, NPS * LP, 0)] + [(NPS * LP + i * 512, 512, 1) for i in range(LS // 512)]
    with em.scope() as es:
        ea = em.sb("ea", [128, 64], F32, es)
        dtb = em.sb("dtb", [128, 64], F32, es)
        em.dma(ea[:], bcast_row(cx.dn_a_log.rearrange("o d h -> o (d h)")))
        em.dma(dtb[:], bcast_row(cx.dn_dt_bias.rearrange("o d h -> o (d h)")))
        em.act(ea[:], ea[:], AF.Exp)
        hT = em.sb("hT", [128, DC, 512], BF16, es)
        cur_var = None
        for (t0, n, var) in groups:
            if var != cur_var:
                gm, sh = make_gm(em, es, cx, 0, var, 0, "n1")
                cur_var = var
            for tt in range(n // 128):
                xt = em.rot(es, "xt", [128, D], F32, 2)
                em.dma(xt[:], x_src(cx, 0, t0 + tt * 128))
                norm_mod_T(em, es, cx, xt, gm, sh, hT, tt * 128)
            for c in range(64):
                if c % 2 == 0:
                    wb = em.rot(es, "winb", [128, DC, 256], BF16, 2)
                    em.load_w(es, wb, cx.dn_w_in[0, :, c * 128:(c + 2) * 128], DC, 256)
                pp = em.ps()
                for dc in range(DC):
                    em.mm(pp[:, 0:n], wb[:, dc, (c % 2) * 128:(c % 2 + 1) * 128], hT[:, dc, 0:n], start=(dc == 0), stop=(dc == DC - 1))
                pj = em.rot(es, "pj", [128, 512], F32, 3)
                em.copy(pj[:, 0:n], pp[:, 0:n], e="act")
                em.dma(cx.projT[c, :, t0:t0 + n], pj[:, 0:n])
            for cc in range(8):
                wz = em.rot(es, "wz", [128, DC, 512], BF16, 2)
                em.load_w(es, wz, cx.dn_w_in[0, :, DN_QKV + cc * 512:DN_QKV + (cc + 1) * 512], DC, 512)
                for tt in range(n // 128):
                    pz = em.ps()
                    for dc in range(DC):
                        em.mm(pz[:], hT[:, dc, tt * 128:(tt + 1) * 128], wz[:, dc, :], start=(dc == 0), stop=(dc == DC - 1))
                    zt = em.rot(es, "zt", [128, 512], BF16, 3)
                    em.act(zt[:], pz[:], AF.Silu)
                    em.dma(cx.z[t0 + tt * 128:t0 + (tt + 1) * 128, cc * 512:(cc + 1) * 512], zt[:])
            wba = em.rot(es, "wba", [128, DC, 128], BF16, 1)
            em.load_w(es, wba, cx.dn_w_in[0, :, DN_QKV + 4096:DN_QKV + 4096 + 128], DC, 128)
            for tt in range(n // 128):
                pb = em.ps()
                for dc in range(DC):
                    em.mm(pb[:, 0:128], hT[:, dc, tt * 128:(tt + 1) * 128], wba[:, dc, :], start=(dc == 0), stop=(dc == DC - 1))
                bg = em.rot(es, "bgt", [128, 128], F32, 2)
                em.act(bg[:, 0:64], pb[:, 0:64], AF.Sigmoid)
                tmp = em.rot(es, "bgtmp", [128, 64], F32, 2)
                em.tt(tmp[:], pb[:, 64:128], dtb[:], ALU.add)
                em.act(tmp[:], tmp[:], AF.Exp)
                em.act(tmp[:], tmp[:], AF.Ln, bias=1.0)
                em.stt(bg[:, 64:128], tmp[:], -1.0, ea[:], ALU.mult, ALU.mult)
                em.dma(cx.bg[t0 + tt * 128:t0 + (tt + 1) * 128, :], bg[:])


def phase_dn_conv(em, cx):
    seqs = [(i * LP, LP) for i in range(NPS)] + [(NPS * LP, LS)]
    with em.scope() as es:
        cw = em.sb("convw", [128, 5, 64], F32, es)
        with cx.nc.allow_non_contiguous_dma("small transposed conv weight load"):
            for k in range(5):
                em.dma(cw[:, k, :], cx.dn_conv_w[0, k, :].rearrange("(c p) -> p c", p=128))
        onesb = em.sb("onesb", [128, 128], BF16, es)
        em.memset(onesb[:], 1.0)
        for (t0, L) in seqs:
            for c in range(64):
                xin = em.rot(es, "cv_in", [128, LS + 4], F32, 2)
                em.memset(xin[:, 0:2], 0.0, e="pool")
                em.memset(xin[:, L + 2:L + 4], 0.0, e="pool")
                em.dma(xin[:, 2:L + 2], cx.projT[c, :, t0:t0 + L])
                acc = em.rot(es, "cv_acc", [128, LS], F32, 2)
                em.ts(acc[:, 0:L], xin[:, 0:L], cw[:, 0, c:c + 1], ALU.mult)
                for k in range(1, 5):
                    em.stt(acc[:, 0:L], xin[:, k:k + L], cw[:, k, c:c + 1], acc[:, 0:L], ALU.mult, ALU.add,
                           e=("dve" if k % 2 else "dve"))
                so = em.rot(es, "cv_so", [128, LS], BF16, 2)
                em.act(so[:, 0:L], acc[:, 0:L], AF.Silu)
                if c < 32:
                    sq = em.rot(es, "cv_sq", [128, LS], BF16, 1)
                    em.tt(sq[:, 0:L], so[:, 0:L], so[:, 0:L], ALU.mult, e="pool")
                    nrm = em.rot(es, "cv_nrm", [128, LS], BF16, 2)
                    for b0 in range(0, L, 512):
                        w = min(512, L - b0)
                        pn = em.ps()
                        em.mm(pn[:, 0:w], onesb[:], sq[:, b0:b0 + w])
                        rn = em.rot(es, "cv_rn", [128, 512], F32, 2)
                        em.ts(rn[:, 0:w], pn[:, 0:w], EPS, ALU.add)
                        em.act(rn[:, 0:w], rn[:, 0:w], AF.Sqrt)
                        em.op("dve", "reciprocal", out=rn[:, 0:w], in_=rn[:, 0:w])
                        if c < 16:
                            em.stt(nrm[:, b0:b0 + w], so[:, b0:b0 + w], 128 ** -0.5, rn[:, 0:w], ALU.mult, ALU.mult)
                        else:
                            em.tt(nrm[:, b0:b0 + w], so[:, b0:b0 + w], rn[:, 0:w], ALU.mult)
                    if c < 16:
                        em.dma(cx.qT[c, :, t0:t0 + L], nrm[:, 0:L])
                    else:
                        em.dma(cx.kT[c - 16, :, t0:t0 + L], nrm[:, 0:L])
                    src = nrm
                else:
                    src = so
                if c >= 16:
                    for b0 in range(0, L, 1024):
                        nt = min(8, (L - b0) // 128)
                        pt = em.ps()
                        ptb = pt[:].bitcast(BF16)
                        for k in range(nt):
                            em.mm(ptb[:, k * 128:(k + 1) * 128], src[:, b0 + k * 128:b0 + (k + 1) * 128], cx.identb[:], transpose=True)
                        tk = em.rot(es, "cv_tk", [128, 8, 128], BF16, 2)
                        em.copy(tk[:, 0:nt, :], ptb[:, 0:nt * 128].rearrange("p (t d) -> p t d", t=nt), e="act")
                        if c < 32:
                            dst = cx.ktok[t0 + b0:t0 + b0 + nt * 128, c - 16, :]
                        else:
                            dst = cx.vtok[t0 + b0:t0 + b0 + nt * 128, c - 32, :]
                        em.dma(dst.rearrange("(t p) d -> p t d", p=128), tk[:, 0:nt, :])


def phase_dn_scan(em, cx):
    seqs = [(i * LP, LP, i) for i in range(NPS)] + [(NPS * LP, LS, -1)]
    with em.scope() as es:
        st = dn_setup(em, es, cx.dncst, 32, False)
        for (t0, L, pi) in seqs:
            nch = L // 128
            for d in range(2):
                for h in range(32):
                    if pi >= 0:
                        em.memset(st.S[d][h][:], 0.0, e="pool")
                        em.memset(st.Sb[d][h][:], 0.0, e="pool")
                    else:
                        em.dma(st.S[d][h][:], cx.s0[d, h], stream="s0ld")
                        em.copy(st.Sb[d][h][:], st.S[d][h][:], e="act")
                order = range(nch) if d == 0 else range(nch - 1, -1, -1)
                for c in order:
                    a = t0 + c * 128
                    kT = em.rot(es, "s_kT", [128, 16, 128], BF16, 2)
                    em.dma(kT[:], cx.kT[:, :, a:a + 128].rearrange("h p t -> p h t"))
                    qT = em.rot(es, "s_qT", [128, 16, 128], BF16, 2)
                    em.dma(qT[:], cx.qT[:, :, a:a + 128].rearrange("h p t -> p h t"))
                    kt = em.rot(es, "s_kt", [128, 16, 128], BF16, 2)
                    em.dma(kt[:], cx.ktok[a:a + 128])
                    vt = em.rot(es, "s_vt", [128, 32, 128], BF16, 2)
                    em.dma(vt[:], cx.vtok[a:a + 128])
                    bgt = em.rot(es, "s_bg", [128, 128], F32, 2)
                    em.dma(bgt[:], cx.bg[a:a + 128, :])
                    oo = em.rot(es, "s_oo", [128, 32, 128], F32, 2)
                    dn_chunk(em, es, st, d, kT, qT, kt, vt, bgt[:, d * 32:(d + 1) * 32], bgt[:, 64 + d * 32:64 + (d + 1) * 32], 2, True, oo)
                    em.dma(cx.odn[d, a:a + 128, :], oo[:].rearrange("p h d -> p (h d)"))
                if pi >= 0:
                    for h in range(32):
                        em.dma(cx.nsd[pi, d, h], st.S[d][h][:], stream="nsdst")


def phase_post(em, cx, layer):
    groups = [(0, NPS * LP, 0)] + [(NPS * LP + i * 512, 512, 1) for i in range(LS // 512)]
    NCH = 32 if layer == 0 else 16
    w_o = cx.dn_w_o[0] if layer == 0 else cx.na_w_o[0]
    with em.scope() as es:
        keysT = peer_keysT(em, es, cx.peer_keys[layer], cx.identf)
        if layer == 0:
            gdn = em.sb("gdn", [128, 128], F32, es)
            em.dma(gdn[:], bcast_row(cx.dn_norm_g[0:1, :]))
        if layer == 1:
            fg = em.sb("fg", [128, D], F32, es)
            em.dma(fg[:], bcast_row(cx.final_g.rearrange("(o d) -> o d", o=1)))
        h2T = em.sb("h2T", [128, DC, 512], BF16, es)
        for (t0, n, var) in groups:
            ntp = n // 128
            with em.scope() as eg:
                x1 = [em.sb("x1", [128, D], F32, eg) for _ in range(ntp)]
                for tt in range(ntp):
                    em.dma(x1[tt][:], x_src(cx, layer, t0 + tt * 128))
                with em.scope() as eb:
                    g1 = load_mod(em, eb, cx, layer, var, 2, "g1")
                    ogT = em.sb("ogT", [128, NCH, 512], BF16, eb)
                    if layer == 0:
                        with em.scope() as ea:
                            for tt in range(ntp):
                                a = t0 + tt * 128
                                of = em.rot(ea, "of", [128, 32, 128], F32, 1)
                                ob = em.rot(ea, "ob", [128, 32, 128], F32, 1)
                                em.dma(of[:], cx.odn[0, a:a + 128, :].rearrange("p (h d) -> p h d", h=32))
                                em.dma(ob[:], cx.odn[1, a:a + 128, :].rearrange("p (h d) -> p h d", h=32))
                                zt = em.rot(ea, "zg", [128, 32, 128], BF16, 1)
                                em.dma(zt[:], cx.z[a:a + 128, :].rearrange("p (h d) -> p h d", h=32))
                                em.tt(of[:], of[:], ob[:], ALU.add, e="pool")
                                em.tt(ob[:], of[:], of[:], ALU.mult)
                                ms = em.rot(ea, "ms", [128, 32], F32, 2)
                                em.op("dve", "tensor_reduce", out=ms[:], in_=ob[:], op=ALU.add, axis=AX.X)
                                em.ts(ms[:], ms[:], 1.0 / 128, ALU.mult, EPS, ALU.add)
                                em.act(ms[:], ms[:], AF.Sqrt)
                                em.op("dve", "reciprocal", out=ms[:], in_=ms[:])
                                em.tt(of[:], of[:], ms[:].unsqueeze(2).to_broadcast([128, 32, 128]), ALU.mult)
                                em.tt(of[:], of[:], gdn[:].unsqueeze(1).to_broadcast([128, 32, 128]), ALU.mult, e="pool")
                                og = em.rot(ea, "og", [128, 32 * 128], BF16, 2)
                                em.tt(og[:].rearrange("p (h d) -> p h d", h=32), of[:], zt[:], ALU.mult)
                                for g0 in range(0, 32, 8):
                                    pt = em.ps()
                                    ptb = pt[:].bitcast(BF16)
                                    for k in range(8):
                                        em.mm(ptb[:, k * 128:(k + 1) * 128], og[:, (g0 + k) * 128:(g0 + k + 1) * 128], cx.identb[:], transpose=True)
                                    em.copy(ogT[:, g0:g0 + 8, tt * 128:(tt + 1) * 128], ptb[:].rearrange("p (c t) -> p c t", c=8), e="act")
                    else:
                        em.dma(ogT[:, :, 0:n], cx.oT[:, :, t0:t0 + n].rearrange("h p t -> p h t"))
                    for cc in range(4):
                        wo = em.rot(eb, "wo", [128, NCH, 512], BF16, 1)
                        for hf in range(2):
                            hs = slice(hf * NCH // 2, (hf + 1) * NCH // 2)
                            em.load_w(eb, wo[:, hs, :], w_o[hf * NCH * 64:(hf + 1) * NCH * 64, cc * 512:(cc + 1) * 512], NCH // 2, 512)
                        for tt in range(ntp):
                            po = em.ps()
                            for ch in range(NCH):
                                em.mm(po[:], ogT[:, ch, tt * 128:(tt + 1) * 128], wo[:, ch, :], start=(ch == 0), stop=(ch == NCH - 1))
                            tmp = em.rot(eb, "potmp", [128, 512], F32, 2)
                            em.tt(tmp[:], po[:], g1[:, cc * 512:(cc + 1) * 512], ALU.mult)
                            em.tt(x1[tt][:, cc * 512:(cc + 1) * 512], x1[tt][:, cc * 512:(cc + 1) * 512], tmp[:], ALU.add, e="pool")
                gm2, sh2 = make_gm(em, eg, cx, layer, var, 1, "n2")
                for tt in range(ntp):
                    norm_mod_T(em, eg, cx, x1[tt], gm2, sh2, h2T, tt * 128)
                    em.dma(cx.xmid[t0 + tt * 128:t0 + (tt + 1) * 128, :], x1[tt][:])
            with em.scope() as ep:
                acc = [em.sb("pacc", [128, D], F32, ep) for _ in range(ntp)]
                peer_pass(em, cx.nc, ep, DC, h2T, ntp, cx.peer_w_q[layer], keysT, cx.peer_u[layer], cx.peer_v[layer], cx.identb, acc)
                g2 = load_mod(em, ep, cx, layer, var, 5, "g2")
                for tt in range(ntp):
                    a = t0 + tt * 128
                    xr = em.rot(ep, "xr", [128, D], F32, 2)
                    em.dma(xr[:], cx.xmid[a:a + 128, :])
                    em.tt(acc[tt][:], acc[tt][:], g2[:], ALU.mult, e="pool")
                    em.tt(xr[:], xr[:], acc[tt][:], ALU.add)
                    if layer == 0:
                        em.dma(cx.xres[0][a:a + 128, :], xr[:])
                    else:
                        junk = em.rot(ep, "fjunk", [128, D], BF16, 1)
                        ss = em.rot(ep, "fss", [128, 1], F32, 2)
                        em.act(junk[:], xr[:], AF.Square, accum_out=ss[:])
                        em.ts(ss[:], ss[:], 1.0 / D, ALU.mult, EPS, ALU.add)
                        em.act(ss[:], ss[:], AF.Sqrt)
                        em.op("dve", "reciprocal", out=ss[:], in_=ss[:])
                        em.stt(acc[tt][:], xr[:], ss[:, 0:1], fg[:], ALU.mult, ALU.mult)
                        if a < NPS * LP:
                            em.dma(cx.yp[a:a + 128, :], acc[tt][:])
                        else:
                            em.dma(cx.ys[a - NPS * LP:a - NPS * LP + 128, :], acc[tt][:])


NA_SCALE = 128 ** -0.5


def attend(em, es, cx, chunks, qT_ap, nq, oT_dst, etab_ap=None):
    nchk = len(chunks)
    pS = em.ps()
    for i, (kt, v) in enumerate(chunks):
        em.mm(pS[:, i * nq:(i + 1) * nq], kt, qT_ap)
    E = em.rot(es, "at_E", [128, 512], BF16, 3)
    em.act(E[:, 0:nchk * nq], pS[:, 0:nchk * nq], AF.Exp, scale=NA_SCALE)
    if etab_ap is not None:
        nl = etab_ap.shape[1]
        v3 = E[:, 0:nl * nq].rearrange("p (m q) -> p m q", m=nl)
        em.tt(v3, v3, etab_ap, ALU.mult)
    pN = em.ps()
    for i, (kt, v) in enumerate(chunks):
        em.mm(pN[:, 0:nq], v, E[:, i * nq:(i + 1) * nq], start=(i == 0), stop=(i == nchk - 1))
    for i in range(nchk):
        em.mm(pN[:, 256:256 + nq], cx.onesb[:], E[:, i * nq:(i + 1) * nq], start=(i == 0), stop=(i == nchk - 1))
    rd = em.rot(es, "at_rd", [128, 256], F32, 3)
    em.op("dve", "reciprocal", out=rd[:, 0:nq], in_=pN[:, 256:256 + nq])
    em.tt(oT_dst, pN[:, 0:nq], rd[:, 0:nq], ALU.mult)


def phase_na(em, cx):
    groups = [(0, NPS * LP, 0)] + [(NPS * LP + i * 512, 512, 1) for i in range(LS // 512)]
    wq = cx.na_w_qkv[0]
    with em.scope() as es:
        hT = em.sb("hT", [128, DC, 512], BF16, es)
        for (t0, n, var) in groups:
            with em.scope() as eg:
                gm, sh = make_gm(em, eg, cx, 1, var, 0, "n1")
                for tt in range(n // 128):
                    xt = em.rot(eg, "xt", [128, D], F32, 2)
                    em.dma(xt[:], x_src(cx, 1, t0 + tt * 128))
                    norm_mod_T(em, eg, cx, xt, gm, sh, hT, tt * 128)
                for c in range(32):
                    if c % 2 == 0:
                        wb = em.rot(eg, "wqkb", [128, DC, 256], BF16, 2)
                        em.load_w(eg, wb, wq[:, c * 128:(c + 2) * 128], DC, 256)
                    pp = em.ps()
                    for dc in range(DC):
                        em.mm(pp[:, 0:n], wb[:, dc, (c % 2) * 128:(c % 2 + 1) * 128], hT[:, dc, 0:n], start=(dc == 0), stop=(dc == DC - 1))
                    pj = em.rot(eg, "pjn", [128, 512], BF16, 3)
                    em.copy(pj[:, 0:n], pp[:, 0:n], e="act")
                    dst = cx.qnT if c < 16 else cx.knT
                    em.dma(dst[c % 16, :, t0:t0 + n], pj[:, 0:n])
                for which in ([2, 1] if var == 0 else [2]):
                    for cc in range(4):
                        wv = em.rot(eg, "wvb", [128, DC, 512], BF16, 2)
                        em.load_w(eg, wv, wq[:, which * D + cc * 512:which * D + (cc + 1) * 512], DC, 512)
                        for tt in range(n // 128):
                            a = t0 + tt * 128
                            pv = em.ps()
                            for dc in range(DC):
                                em.mm(pv[:], hT[:, dc, tt * 128:(tt + 1) * 128], wv[:, dc, :], start=(dc == 0), stop=(dc == DC - 1))
                            if which == 2:
                                vb = em.rot(eg, "vnb", [128, 512], BF16, 3)
                                em.copy(vb[:], pv[:], e="act")
                                em.dma(cx.vn[a:a + 128, cc * 512:(cc + 1) * 512], vb[:])
                            if var == 0:
                                vf = em.rot(eg, "vnf", [128, 512], F32, 3)
                                em.ts(vf[:], pv[:], 1.0, ALU.mult)
                                dst = cx.ncv if which == 2 else cx.nck
                                em.dma(dst[a:a + 128, cc * 512:(cc + 1) * 512], vf[:])
    with em.scope() as es:
        for s in range(NPS):
            t0 = s * LP
            for h in range(16):
                kt = em.rot(es, "c_kt", [128, LP], BF16, 2)
                em.dma(kt[:], cx.knT[h, :, t0:t0 + LP])
                qt = em.rot(es, "c_qt", [128, LP], BF16, 2)
                em.dma(qt[:], cx.qnT[h, :, t0:t0 + LP])
                vv = em.rot(es, "c_v", [128, LP // 128, 128], BF16, 2)
                em.dma(vv[:], cx.vn[t0:t0 + LP, h * 128:(h + 1) * 128].rearrange("(t p) d -> p t d", p=128))
                ot = em.rot(es, "c_ot", [128, LP], BF16, 2)
                chunks = [(kt[:, kc * 128:(kc + 1) * 128], vv[:, kc, :]) for kc in range(LP // 128)]
                attend(em, es, cx, chunks, qt[:, :], LP, ot[:, :])
                em.dma(cx.oT[h, :, t0:t0 + LP], ot[:])
    with em.scope() as es:
        t0 = NPS * LP
        kcT = em.sb("kcT", [128, 16, 256], BF16, es)
        vc = em.sb("vc", [128, 2, 16, 128], BF16, es)
        for tt in range(2):
            ck = em.rot(es, "ckf", [128, 16, 128], F32, 2)
            em.dma(ck[:], cx.ck[tt * 128:(tt + 1) * 128])
            em.dma(vc[:, tt], cx.cv[tt * 128:(tt + 1) * 128], q="pool")
            for g0 in range(0, 16, 4):
                pt = em.ps()
                for k in range(4):
                    em.mm(pt[:, k * 128:(k + 1) * 128], ck[:, g0 + k, :], cx.identf[:], transpose=True)
                em.copy(kcT[:, g0:g0 + 4, tt * 128:(tt + 1) * 128], pt[:].rearrange("p (h t) -> p h t", h=4), e="act")
        okm = em.sb("okm", [128, 64], F32, es)
        em.dma(okm[:], cx.okm)
        for h in range(16):
            rp = em.rot(es, "rpx", [128, 14, 64], F32, 2)
            em.dma(rp[:], cx.rpbx[:, h])
            em.act(rp[:], rp[:], AF.Exp)
            etab = em.rot(es, "etab", [128, 14, 64], BF16, 2)
            em.tt(etab[:], rp[:], okm[:].unsqueeze(1).to_broadcast([128, 14, 64]), ALU.mult)
            kt = em.rot(es, "l_kt", [128, LS], BF16, 2)
            em.dma(kt[:], cx.knT[h, :, t0:t0 + LS])
            qt = em.rot(es, "l_qt", [128, LS], BF16, 2)
            em.dma(qt[:], cx.qnT[h, :, t0:t0 + LS])
            ve = em.rot(es, "l_ve", [128, 32, 128], BF16, 2)
            em.dma(ve[:], cx.vn[t0:t0 + LS, h * 128:(h + 1) * 128].rearrange("(t p) d -> p t d", p=128))
            vo = em.rot(es, "l_vo", [128, 31, 128], BF16, 2)
            em.dma(vo[:], cx.vn[t0 + 64:t0 + LS - 64, h * 128:(h + 1) * 128].rearrange("(t p) d -> p t d", p=128))
            ot = em.rot(es, "l_ot", [128, LS], BF16, 2)
            for r in range(64):
                r0 = min(max(r - 4, 0), 56)
                chunks = []
                for m in range(4):
                    a = (r0 + 2 * m) * 64
                    vsrc = ve[:, a // 128, :] if a % 128 == 0 else vo[:, (a - 64) // 128, :]
                    chunks.append((kt[:, a:a + 128], vsrc))
                for kc in range(2):
                    chunks.append((kcT[:, h, kc * 128:(kc + 1) * 128], vc[:, kc, h, :]))
                base = r0 - r + 7
                attend(em, es, cx, chunks, qt[:, r * 64:(r + 1) * 64], 64, ot[:, r * 64:(r + 1) * 64],
                       etab_ap=etab[:, base:base + 7:2, :])
            em.dma(cx.oT[h, :, t0:t0 + LS], ot[:])


_IN_SPECS = [
    ("xp", [NPS * LP, D], F32), ("xs", [LS, D], F32), ("cvec", [2, D], F32),
    ("s0", [2, 32, 128, 128], F32), ("ck", [256, 16, 128], F32), ("cv", [256, 16, 128], F32),
    ("ada_w", [2, D, 6 * D], F32), ("ada_b", [2, 6 * D], F32), ("norm1_g", [2, D], F32), ("norm2_g", [2, D], F32),
    ("final_g", [D], F32), ("dn_w_in", [1, D, 12416], F32), ("dn_conv_w", [1, 5, 8192], F32),
    ("dn_a_log", [1, 2, 32], F32), ("dn_dt_bias", [1, 2, 32], F32), ("dn_norm_g", [1, 128], F32),
    ("dn_w_o", [1, 4096, D], F32), ("na_w_qkv", [1, D, 3 * D], F32), ("na_w_o", [1, D, D], F32),
    ("peer_w_q", [2, D, 2048], F32), ("peer_keys", [2, 8, 2, 128, 128], F32),
    ("peer_u", [2, 16384, D], F32), ("peer_v", [2, 16384, D], F32),
    ("dncst", [128, 6, 128], F32), ("rpbx", [128, 16, 14, 64], F32), ("okm", [128, 64], F32),
]
_OUT_SPECS = [
    ("yp", [NPS * LP, D], F32), ("ys", [LS, D], F32), ("nsd", [NPS, 2, 32, 128, 128], F32),
    ("nck", [NPS * LP, D], F32), ("ncv", [NPS * LP, D], F32),
]


def build_nc(stop_after=99):
    nc = bass.Bass("TRN2", target_bir_lowering=False)
    cx = Ctx()
    cx.nc = nc
    for name, shape, dt in _IN_SPECS:
        setattr(cx, name, nc.dram_tensor(name, shape, dt, kind="ExternalInput").ap())
    for name, shape, dt in _OUT_SPECS:
        setattr(cx, name, nc.dram_tensor(name, shape, dt, kind="ExternalOutput").ap())
    cx.mod = dram(nc, "mod", [2, 2, 6 * D], F32)
    cx.projT = dram(nc, "projT", [64, 128, LTOT], F32)
    cx.z = dram(nc, "zs", [LTOT, 4096], BF16)
    cx.bg = dram(nc, "bg", [LTOT, 128], F32)
    cx.qT = dram(nc, "qT", [16, 128, LTOT], BF16)
    cx.kT = dram(nc, "kT", [16, 128, LTOT], BF16)
    cx.ktok = dram(nc, "ktok", [LTOT, 16, 128], BF16)
    cx.vtok = dram(nc, "vtok", [LTOT, 32, 128], BF16)
    cx.odn = dram(nc, "odn", [2, LTOT, 4096], F32)
    cx.xres = [dram(nc, "xres0", [LTOT, D], F32)]
    cx.xmid = dram(nc, "xmid", [LTOT, D], F32)
    cx.qnT = dram(nc, "qnT", [16, 128, LTOT], BF16)
    cx.knT = dram(nc, "knT", [16, 128, LTOT], BF16)
    cx.vn = dram(nc, "vn", [LTOT, D], BF16)
    cx.oT = dram(nc, "oT", [16, 128, LTOT], BF16)
    with contextlib.ExitStack() as es:
        em = Em(nc, es)
        em.init_psum()
        cx.identf = em.sb("identf", [128, 128], F32)
        em.dma(cx.identf[:], cx.dncst[:, 0, :])
        cx.identb = em.sb("identb", [128, 128], BF16)
        em.copy(cx.identb[:], cx.identf[:])
        cx.onesb = em.sb("onesb", [128, 128], BF16)
        em.memset(cx.onesb[:], 1.0)
        phase_adaln(em, cx)
        if stop_after >= 1:
            phase_dn_proj(em, cx)
            phase_dn_conv(em, cx)
            phase_dn_scan(em, cx)
        if stop_after >= 2:
            phase_post(em, cx, 0)
        if stop_after >= 3:
            phase_na(em, cx)
        if stop_after >= 4:
            phase_post(em, cx, 1)
        em.barrier()
        cx.n_inst = em.n_inst
    return nc, cx


def _rpb_layout(rpb):
    kc = np.arange(64)[:, None]
    qc = np.arange(64)[None, :]
    dc = np.clip(kc - qc, -15, 15) + 15
    out = np.empty((2, 64, 16, 14, 64), np.float32)
    for j in range(2):
        g = rpb[:, j:j + 14, :]
        out[j] = np.transpose(g[:, :, dc], (2, 0, 1, 3))
    return np.ascontiguousarray(out.reshape(128, 16, 14, 64))


def _ok_mask():
    qc = np.arange(64)
    c0 = np.clip(qc - 8, 0, 48)
    kc = np.arange(64)[:, None]
    ok = ((kc >= c0[None, :]) & (kc < c0[None, :] + 16)).astype(np.float32)
    return np.ascontiguousarray(np.concatenate([ok, ok], axis=0))


_NC_CACHE = {}


def kernel(x_prompt, x_sample, c, state_delta, cache_k, cache_v, c_ctx, ada_w, ada_b, norm1_g, norm2_g,
           final_g, dn_w_in, dn_conv_w, dn_a_log, dn_dt_bias, dn_norm_g, dn_w_o, na_w_qkv, na_rpb, na_w_o,
           peer_w_q, peer_keys, peer_u, peer_v):
    f = lambda a: np.ascontiguousarray(np.asarray(a, dtype=np.float32))
    x_prompt, x_sample, c, state_delta, cache_k, cache_v, c_ctx = map(f, (x_prompt, x_sample, c, state_delta, cache_k, cache_v, c_ctx))
    shared = dict(ada_w=f(ada_w), ada_b=f(ada_b), norm1_g=f(norm1_g), norm2_g=f(norm2_g), final_g=f(final_g),
                  dn_w_in=f(dn_w_in), dn_conv_w=f(dn_conv_w), dn_a_log=f(dn_a_log), dn_dt_bias=f(dn_dt_bias),
                  dn_norm_g=f(dn_norm_g), dn_w_o=f(dn_w_o), na_w_qkv=f(na_w_qkv), na_w_o=f(na_w_o),
                  peer_w_q=f(peer_w_q), peer_keys=f(peer_keys), peer_u=f(peer_u), peer_v=f(peer_v),
                  dncst=dn_host_consts(), rpbx=_rpb_layout(f(na_rpb)[0]), okm=_ok_mask())
    if "nc" not in _NC_CACHE:
        _NC_CACHE["nc"] = build_nc()
    nc, cx = _NC_CACHE["nc"]
    in_maps = []
    for core in range(8):
        b = core % 2
        m = dict(shared)
        m["xp"] = np.ascontiguousarray(x_prompt[core * NPS:(core + 1) * NPS].reshape(NPS * LP, D))
        m["xs"] = x_sample[b]
        m["cvec"] = np.ascontiguousarray(np.stack([c_ctx, c[b]], axis=0))
        m["s0"] = np.ascontiguousarray(state_delta[b, 0])
        m["ck"] = np.ascontiguousarray(cache_k[b, 0])
        m["cv"] = np.ascontiguousarray(cache_v[b, 0])
        in_maps.append(m)
    res = run_bass_kernel_spmd(nc, in_maps, core_ids=list(range(8)))
    r = res.results
    y_prompt = np.concatenate([r[i]["yp"].reshape(NPS, LP, D) for i in range(8)], axis=0)
    y_sample = np.stack([r[0]["ys"], r[1]["ys"]], axis=0)
    nsd = np.concatenate([r[i]["nsd"] for i in range(8)], axis=0)[:, None]
    nck = np.concatenate([r[i]["nck"].reshape(NPS, LP, 16, 128) for i in range(8)], axis=0)[:, None]
    ncv = np.concatenate([r[i]["ncv"].reshape(NPS, LP, 16, 128) for i in range(8)], axis=0)[:, None]
    return (y_prompt.astype(np.float32), y_sample.astype(np.float32), nsd.astype(np.float32),
            nck.astype(np.float32), ncv.astype(np.float32))
```
